# Optimizing a Trainium2 kernel written in Bass

```python
import jax, jax.numpy as jnp
from jax import lax
import numpy as np

D_MODEL = 2048
BATCH = 8
SEQ = 2048
DEPTH = 1

CHUNK = 64
N_META = 16
D_MIX = D_MODEL
D_CONV = D_MIX // 2
CONV_GROUPS = 8
CONV_K = 3
D_ATTN = D_MIX - D_CONV
N_ATTN_HEADS = 8
ATTN_HEAD_DIM = D_ATTN // N_ATTN_HEADS
HEAD_DIM_OUT = D_CONV // CONV_GROUPS
Q_BLOCK = 128
D_IN_PROJ = 3 * D_CONV + 3 * D_ATTN
N_PEER_HEADS = 8
PEER_TOPK = 16
N_KEYS = 128
N_EXPERTS = N_KEYS * N_KEYS
D_QUERY = 256
D_SUBKEY = D_QUERY // 2
PEER_TOKEN_BLOCK = 128
EPS = 1e-6

kernel_name = "hybrid_conv_stickbreak_peer"


def rmsnorm(x, g):
    xf = x.astype(jnp.float32)
    y = xf * lax.rsqrt(jnp.mean(xf * xf, axis=-1, keepdims=True) + EPS)
    return (y * g.astype(jnp.float32)).astype(x.dtype)


def short_gated_conv(gate_b, gate_c, conv_in, conv_w):
    t = conv_in.shape[1]
    u = gate_c * conv_in
    up = jnp.pad(u, ((0, 0), (CONV_K - 1, 0), (0, 0)))
    y = up[:, 0:t] * conv_w[0]
    for j in range(1, CONV_K):
        y = y + up[:, j:j + t] * conv_w[j]
    return gate_b * y


def stick_breaking_attention(q, k, v):
    b, nh, t, dh = q.shape
    t_pad = -(-t // Q_BLOCK) * Q_BLOCK
    pad = ((0, 0), (0, 0), (0, t_pad - t), (0, 0))
    q, k, v = jnp.pad(q, pad), jnp.pad(k, pad), jnp.pad(v, pad)
    scale = dh ** -0.5
    outs = []
    for blk in range(t_pad // Q_BLOCK):
        q_lo = blk * Q_BLOCK
        kv_len = q_lo + Q_BLOCK
        qb = q[:, :, q_lo:kv_len]
        kb = k[:, :, :kv_len]
        vb = v[:, :, :kv_len]
        z = jnp.einsum('bhqd,bhkd->bhqk', qb, kb).astype(jnp.float32) * scale
        q_pos = q_lo + jnp.arange(Q_BLOCK)[:, None]
        k_pos = jnp.arange(kv_len)[None, :]
        past = k_pos < q_pos
        log_not = jnp.where(past, jax.nn.log_sigmoid(-z), 0.0)
        rest = lax.cumsum(log_not, axis=3, reverse=True) - log_not
        w = jnp.where(past, jnp.exp(jax.nn.log_sigmoid(z) + rest), 0.0)
        outs.append(jnp.einsum('bhqk,bhkd->bhqd', w.astype(vb.dtype), vb))
    return jnp.concatenate(outs, axis=2)[:, :, :t]


def mixer_sublayer(h, norm_g, w_in, conv_w, q_norm_g, k_norm_g, out_norm_g, w_out):
    b, t, _ = h.shape
    xn = rmsnorm(h, norm_g)
    proj = xn @ w_in
    gate_b, gate_c, conv_in, q, k, v = jnp.split(
        proj, [D_CONV, 2 * D_CONV, 3 * D_CONV, 3 * D_CONV + D_ATTN, 3 * D_CONV + 2 * D_ATTN], axis=-1)
    conv_out = short_gated_conv(gate_b, gate_c, conv_in, conv_w)
    def heads(a):
        return a.reshape(b, t, N_ATTN_HEADS, ATTN_HEAD_DIM)
    qh = rmsnorm(heads(q), q_norm_g).transpose(0, 2, 1, 3)
    kh = rmsnorm(heads(k), k_norm_g).transpose(0, 2, 1, 3)
    vh = heads(v).transpose(0, 2, 1, 3)
    attn_out = stick_breaking_attention(qh, kh, vh).transpose(0, 2, 1, 3)
    groups = jnp.concatenate([conv_out.reshape(b, t, CONV_GROUPS, HEAD_DIM_OUT), attn_out], axis=2)
    groups = rmsnorm(groups, jnp.ones((HEAD_DIM_OUT,), jnp.float32))
    mixed = groups.reshape(b, t, D_MIX) * out_norm_g
    return mixed @ w_out


def peer_sublayer(h, norm_g, w_query, sub_keys, expert_u, expert_v):
    b, t, d = h.shape
    n = b * t
    n_pad = -(-n // PEER_TOKEN_BLOCK) * PEER_TOKEN_BLOCK
    tok = jnp.pad(rmsnorm(h, norm_g).reshape(n, d), ((0, n_pad - n), (0, 0)))
    qry = (tok @ w_query).reshape(n_pad, N_PEER_HEADS, 2, D_SUBKEY)
    scores = jnp.einsum('nhpc,pkc->nhpk', qry, sub_keys).astype(jnp.float32)
    s_top, i_top = lax.top_k(scores, PEER_TOPK)
    cand_score = s_top[:, :, 0, :, None] + s_top[:, :, 1, None, :]
    cand_idx = i_top[:, :, 0, :, None] * N_KEYS + i_top[:, :, 1, None, :]
    best, pos = lax.top_k(cand_score.reshape(n_pad, N_PEER_HEADS, PEER_TOPK * PEER_TOPK), PEER_TOPK)
    experts = jnp.take_along_axis(cand_idx.reshape(n_pad, N_PEER_HEADS, PEER_TOPK * PEER_TOPK), pos, axis=-1)
    gates = jax.nn.softmax(best, axis=-1).astype(tok.dtype)

    def block_fn(args):
        xb, eb, gb = args
        u = expert_u[eb]
        vv = expert_v[eb]
        act = jax.nn.gelu(jnp.einsum('thkd,td->thk', u, xb), approximate=False)
        return jnp.einsum('thk,thkd->td', gb * act, vv)

    nb = n_pad // PEER_TOKEN_BLOCK
    out = lax.map(block_fn, (tok.reshape(nb, PEER_TOKEN_BLOCK, d),
                             experts.reshape(nb, PEER_TOKEN_BLOCK, N_PEER_HEADS, PEER_TOPK),
                             gates.reshape(nb, PEER_TOKEN_BLOCK, N_PEER_HEADS, PEER_TOPK)))
    return out.reshape(n_pad, d)[:n].reshape(b, t, d)


def setup_inputs(seed: int = 0) -> dict:
    key = jax.random.key(seed)
    ks = jax.random.split(key, 16)
    f32 = jnp.float32

    def nrm(k, shape, scale):
        return jax.random.normal(k, shape, f32) * scale

    return {
        "x": nrm(ks[0], (BATCH, SEQ, D_MODEL), 1.0),
        "meta_tokens": nrm(ks[1], (N_META, D_MODEL), 1.0),
        "norm_mix_g": 1.0 + nrm(ks[2], (DEPTH, D_MODEL), 0.02),
        "w_in": nrm(ks[3], (DEPTH, D_MODEL, D_IN_PROJ), D_MODEL ** -0.5),
        "conv_w": nrm(ks[4], (DEPTH, CONV_K, D_CONV), CONV_K ** -0.5),
        "q_norm_g": 1.0 + nrm(ks[5], (DEPTH, ATTN_HEAD_DIM), 0.02),
        "k_norm_g": 1.0 + nrm(ks[6], (DEPTH, ATTN_HEAD_DIM), 0.02),
        "out_norm_g": 1.0 + nrm(ks[7], (DEPTH, D_MIX), 0.02),
        "w_out": nrm(ks[8], (DEPTH, D_MIX, D_MODEL), D_MIX ** -0.5),
        "norm_ffn_g": 1.0 + nrm(ks[9], (DEPTH, D_MODEL), 0.02),
        "w_query": nrm(ks[10], (DEPTH, D_MODEL, N_PEER_HEADS * D_QUERY), D_MODEL ** -0.5),
        "sub_keys": nrm(ks[11], (DEPTH, 2, N_KEYS, D_SUBKEY), D_SUBKEY ** -0.5),
        "expert_u": nrm(ks[12], (DEPTH, N_EXPERTS, D_MODEL), D_MODEL ** -0.5),
        "expert_v": nrm(ks[13], (DEPTH, N_EXPERTS, D_MODEL), N_PEER_HEADS ** -0.5),
    }


def reference(x, meta_tokens, norm_mix_g, w_in, conv_w, q_norm_g, k_norm_g, out_norm_g, w_out,
              norm_ffn_g, w_query, sub_keys, expert_u, expert_v):
    b = x.shape[0]
    meta = jnp.broadcast_to(meta_tokens.astype(x.dtype)[None], (b, N_META, D_MODEL))
    h = jnp.concatenate([meta, x], axis=1)
    for layer in range(DEPTH):
        h = h + mixer_sublayer(h, norm_mix_g[layer], w_in[layer], conv_w[layer], q_norm_g[layer],
                               k_norm_g[layer], out_norm_g[layer], w_out[layer])
        h = h + peer_sublayer(h, norm_ffn_g[layer], w_query[layer], sub_keys[layer],
                              expert_u[layer], expert_v[layer])
    return h[:, N_META:]
```

```python
import numpy as np
import ml_dtypes
import concourse.bass as bass
import concourse.mybir as mybir
from concourse.bass_utils import run_bass_kernel_spmd

F32 = mybir.dt.float32
BF16 = mybir.dt.bfloat16
I32 = mybir.dt.int32
U32 = mybir.dt.uint32
AF = mybir.ActivationFunctionType
ALU = mybir.AluOpType
AX = mybir.AxisListType

EPOCH = 16000
N_DMA_SEMS = {"sp": 12, "pool": 28, "act": 4, "pe": 1, "dve": 1}
EPS = 1e-6
NEG = -30000.0
SB_BASE = 16640
SB_END = 229376


class Buf:
    __slots__ = ("name", "w", "r")

    def __init__(self, name):
        self.name = name
        self.w = None
        self.r = {}


class Prog:
    ENGS = ("pe", "act", "dve", "pool", "sp")

    def __init__(self, nc):
        self.nc = nc
        self.ops = {e: [] for e in self.ENGS}
        self.seq = {e: 0 for e in self.ENGS}
        self.known = {e: {} for e in self.ENGS}
        self.sems = {}
        self.dma_rr = {e: 0 for e in self.ENGS}
        self.dma_cnt = {}
        self._sem_ctx = []

    def _sem(self, name):
        if name not in self.sems:
            ctx = self.nc.semaphore(name)
            h = ctx.__enter__()
            self._sem_ctx.append(ctx)
            self.sems[name] = h
        return self.sems[name]

    def _need(self, eng, ev, waits):
        if ev is None:
            return
        key, val = ev
        if self.known[eng].get(key, 0) >= val:
            return
        self.known[eng][key] = val
        waits[key] = max(waits.get(key, 0), val)

    def _deps(self, eng, reads, writes):
        waits = {}
        for b in reads:
            self._need(eng, b.w, waits)
        own = "c_pe_" if eng == "pe" else "#"
        for b in writes:
            if b.w is not None and not b.w[0].startswith(own):
                self._need(eng, b.w, waits)
            for ev in b.r.values():
                if not ev[0].startswith(own):
                    self._need(eng, ev, waits)
        return waits

    def _commit(self, ev, reads, writes):
        k = ev[0]
        for b in reads:
            old = b.r.get(k)
            if old is None or old[1] < ev[1]:
                b.r[k] = ev
        for b in writes:
            b.w = ev
            b.r = {}

    def op(self, eng, emit, reads=(), writes=()):
        waits = self._deps(eng, reads, writes)
        s = self.seq[eng]
        self.seq[eng] = s + 1
        key = "c_%s_%d" % (eng, s // EPOCH)
        ev = (key, s % EPOCH + 1)
        self._sem(key)
        self.ops[eng].append((waits, emit, (key, 1)))
        self._commit(ev, reads, writes)
        return ev

    def dma(self, eng, emit, reads=(), writes=()):
        i = self.dma_rr[eng]
        self.dma_rr[eng] = (i + 1) % N_DMA_SEMS[eng]
        key = "d_%s_%d" % (eng, i)
        self._sem(key)
        cnt = self.dma_cnt.get(key, 0)
        waits = self._deps(eng, reads, writes)
        if cnt > 0:
            self._need(eng, (key, 16 * cnt), waits)
        self.dma_cnt[key] = cnt + 1
        ev = (key, 16 * (cnt + 1))
        self.ops[eng].append((waits, emit, (key, 16)))
        self._commit(ev, reads, writes)
        return ev

    def barrier(self):
        last = {}
        for e in self.ENGS:
            s = self.seq[e]
            if s > 0:
                last["c_%s_%d" % (e, (s - 1) // EPOCH)] = (s - 1) % EPOCH + 1
        for key, cnt in self.dma_cnt.items():
            last[key] = 16 * cnt
        for e in self.ENGS:
            waits = {}
            for key, val in last.items():
                self._need(e, (key, val), waits)
            if waits:
                self.ops[e].append((waits, None, None))

    def emit_all(self):
        nc = self.nc
        with nc.Block() as block:
            def run(engname):
                def body(e):
                    for waits, emit, inc in self.ops[engname]:
                        for key, val in waits.items():
                            e.wait_ge(self.sems[key], val)
                        if emit is not None:
                            emit(e).then_inc(self.sems[inc[0]], inc[1])
                return body
            block.tensor(run("pe"))
            block.scalar(run("act"))
            block.vector(run("dve"))
            block.gpsimd(run("pool"))
            block.sync(run("sp"))


def build(stage=99, with_experts=True):
    nc = bass.Bass("TRN2", target_bir_lowering=False)
    pg = Prog(nc)
    dbg = {}

    def din(name, shape, dt=F32):
        return nc.dram_tensor(name, list(shape), dt, kind="ExternalInput").ap()

    x_d = din("x", [2048, 2048])
    meta_d = din("meta", [16, 2048])
    gmix_d = din("gmix", [1, 2048])
    gffn_d = din("gffn", [1, 2048])
    win_d = din("w_in_b", [48, 128, 16, 128])
    cw_d = din("cw", [128, 24])
    gq_d = din("gq", [128, 1])
    gk_d = din("gk", [128, 1])
    ong_d = din("ong", [128, 16])
    wout_d = din("w_out_b", [128, 16, 2048])
    wq_d = din("w_query_b", [128, 16, 2048])
    skT_d = din("skT", [128, 256])
    cbf_d = din("cbf", [128, 384 + 2048], BF16)
    cf32_d = din("cf32", [128, 128 + 16])
    if with_experts:
        eu_d = din("expert_u", [16384, 2048])
        ev_d = din("expert_v", [16384, 2048])
    out_d = nc.dram_tensor("out", [2048, 2048], F32, kind="ExternalOutput").ap()
    if with_experts:
        ec_d = nc.dram_tensor("euv_bf", [16384, 2, 2048], BF16, kind="Internal").ap()
    EUB = [Buf("eub%d" % i) for i in range(8)]
    EVB = [Buf("evb%d" % i) for i in range(8)]

    def convert(src, which, bufs, c):
        if with_experts:
            pg.dma("pool", lambda e: e.dma_start(out=ec_d[2048 * c:2048 * (c + 1), which, :], in_=src[2048 * c:2048 * (c + 1), :]), [], [bufs[c]])

    cur = [SB_BASE]

    def alloc(name, shape, dt, at=None):
        esz = 2 if dt == BF16 else 4
        nbytes = int(np.prod(shape[1:])) * esz
        off = cur[0] if at is None else at
        t = nc.alloc_sbuf_tensor_at(name, list(shape), dt, offset=off)
        end = off + (nbytes + 31) // 32 * 32
        assert end <= SB_END, (name, end)
        if at is None:
            cur[0] = end
        return t

    cbf = alloc("cbf", [128, 384 + 2048], BF16)
    cf32 = alloc("cf32", [128, 144], F32)
    eps_t = alloc("eps_t", [128, 1], F32)
    cw = alloc("cw_t", [128, 24], F32)
    gq = alloc("gq_t", [128, 1], F32)
    gk = alloc("gk_t", [128, 1], F32)
    ong = alloc("ong_t", [128, 16], F32)
    skT = alloc("skT_t", [128, 256], F32)
    rstd2 = alloc("rstd2", [128, 16], F32)
    idx_all = alloc("idx_all", [128, 16, 128], I32)
    gates_all = alloc("gates_all", [128, 16, 128], F32)
    ident = cbf[:, 0:128]
    negtri = cbf[:, 128:256]
    negones = cbf[:, 256:384]
    masks = cbf[:, 384:384 + 2048].rearrange("p (j q) -> p j q", j=4)
    ones32 = cf32[:, 0:128]
    iota16 = cf32[:, 128:144]
    R_A = cur[0]
    hT = alloc("hT", [128, 16, 2064], BF16)
    R_B = cur[0]
    actT = alloc("actT", [128, 16, 16, 128], BF16)
    R_S = cur[0]
    CONSTB = Buf("const")
    HT = [Buf("hT%d" % i) for i in range(17)]
    ACTB = [Buf("actT%d" % j) for j in range(16)]
    OUTB = [Buf("out%d" % j) for j in range(16)]
    RSTD2 = Buf("rstd2")
    IDXB = [Buf("idx%d" % j) for j in range(16)]
    GATB = [Buf("gat%d" % j) for j in range(16)]

    pb = []
    for i in range(8):
        ctx = nc.psum_tensor("pb%d" % i, [128, 512], F32)
        pb.append(ctx.__enter__())
    PB = [Buf("pb%d" % i) for i in range(8)]

    def dma(eng, out, in_, R=(), W=()):
        return pg.dma(eng, lambda e: e.dma_start(out=out, in_=in_), R, W)

    def mm(out, lhsT, rhs, start, stop, R, W, skip=False):
        return pg.op("pe", lambda e: e.matmul(out, lhsT=lhsT, rhs=rhs, start=start, stop=stop, skip_group_check=skip), R, W)

    def tr(out, in_, idn, R, W):
        return pg.op("pe", lambda e: e.transpose(out=out, in_=in_, identity=idn), R, W)

    def act(out, in_, func, R, W, **kw):
        return pg.op("act", lambda e: e.activation(out=out, in_=in_, func=func, **kw), R, W)

    def vop(eng, name, R, W, **kw):
        return pg.op(eng, lambda e: getattr(e, name)(**kw), R, W)

    dma("sp", cbf[:], cbf_d, W=[CONSTB])
    dma("sp", cf32[:], cf32_d, W=[CONSTB])
    dma("sp", cw[:], cw_d, W=[CONSTB])
    dma("sp", gq[:], gq_d, W=[CONSTB])
    dma("sp", gk[:], gk_d, W=[CONSTB])
    dma("sp", ong[:], ong_d, W=[CONSTB])
    dma("sp", skT[:], skT_d, W=[CONSTB])
    vop("dve", "memset", [], [CONSTB], ap=eps_t[:], constant=EPS)
    pg.barrier()
    vop("dve", "tensor_scalar", [CONSTB], [CONSTB], out=gq[:], in0=gq[:], scalar1=float(128 ** -0.5), scalar2=None, op0=ALU.mult)
    pg.barrier()

    cur[0] = R_S
    xt = [alloc("xt%d" % i, [128, 2048], F32) for i in range(2)]
    XT = [Buf("xt%d" % i) for i in range(2)]
    xs2 = [alloc("xs_%d" % i, [128, 2048], BF16) for i in range(2)]
    XS2 = [Buf("xs_%d" % i) for i in range(2)]
    junk = alloc("junk", [128, 2048], BF16)
    JUNK = Buf("junk")
    gvec = alloc("gvec", [128, 2048], F32)
    GVEC = Buf("gvec")
    ss = [alloc("ss%d" % i, [128, 4], F32) for i in range(2)]
    SS = [Buf("ss%d" % i) for i in range(2)]

    dma("sp", gvec[:], gmix_d.partition_broadcast(128), W=[GVEC])
    tiles = [(0, 16, 0, meta_d)] + [(1 + j, 128, 16 + 128 * j, x_d[128 * j:128 * (j + 1), :]) for j in range(16)]
    for ti, rows, t0, src in tiles:
        k = ti % 2
        dma("sp", xt[k][:rows, :], src, W=[XT[k]])
        act(junk[:rows, :], xt[k][:rows, :], AF.Square, [XT[k]], [JUNK, SS[k]], accum_out=ss[k][:rows, 0:1])
        act(ss[k][:rows, 1:2], ss[k][:rows, 0:1], AF.Sqrt, [SS[k], CONSTB], [SS[k]], bias=eps_t[:rows, :], scale=1.0 / 2048)
        vop("dve", "reciprocal", [SS[k]], [SS[k]], out=ss[k][:rows, 2:3], in_=ss[k][:rows, 1:2])
        xs, XS = xs2[k], XS2[k]
        vop("dve", "scalar_tensor_tensor", [XT[k], SS[k], GVEC], [XS], out=xs[:rows, :], in0=xt[k][:rows, :],
            scalar=ss[k][:rows, 2:3], in1=gvec[:rows, :], op0=ALU.mult, op1=ALU.mult)
        for half in range(2):
            bi = 2 * k + half
            bank = pb[bi][:, :].bitcast(BF16).rearrange("p (a b) -> p a b", a=8)
            for c in range(8):
                cc = half * 8 + c
                tr(bank[:, c, :rows], xs[:rows, cc * 128:(cc + 1) * 128], ident[:rows, :rows], [XS, CONSTB], [PB[bi]])
            dst = hT[:, half * 8:(half + 1) * 8, t0:t0 + rows]
            if half == 0:
                act(dst, bank[:, :, :rows], AF.Copy, [PB[bi]], [HT[ti]])
            else:
                vop("dve", "tensor_copy", [PB[bi]], [HT[ti]], out=dst, in_=bank[:, :, :rows])
    if stage == 1:
        return finish(nc, pg, dbg, {"hT": (hT, BF16, [128, 16, 2064])}, alloc, cur, R_S, dma)
    pg.barrier()

    cur[0] = R_S
    NWB = 6
    wb = [alloc("wb%d" % i, [128, 16, 128], BF16) for i in range(NWB)]
    WB = [Buf("wb%d" % i) for i in range(NWB)]
    wcnt = [0]

    def load_w(blk):
        i = wcnt[0] % NWB
        wcnt[0] += 1
        dma("pool", wb[i][:], win_d[blk], W=[WB[i]])
        return wb[i], WB[i]

    S2 = cur[0]
    gcs = [alloc("gcs%d" % i, [128, 512], F32) for i in range(2)]
    ut = [alloc("ut%d" % i, [128, 516], F32) for i in range(2)]
    yt = [alloc("yt%d" % i, [128, 512], F32) for i in range(2)]
    sq = [alloc("sq%d" % i, [128, 512], BF16) for i in range(2)]
    sd = [alloc("sd%d" % i, [128, 512], F32) for i in range(2)]
    GCS = [Buf("gcs%d" % i) for i in range(2)]
    UT = [Buf("ut%d" % i) for i in range(2)]
    YT = [Buf("yt%d" % i) for i in range(2)]
    SQ = [Buf("sq%d" % i) for i in range(2)]
    SD = [Buf("sd%d" % i) for i in range(2)]

    pieces = [(0, 16, [0])] + [(16 + 512 * n, 512, [1 + 4 * n + i for i in range(4)]) for n in range(4)]
    pcount = [0]

    def rms_part(src_ap, src_bufs, k, N, nbank):
        act(sq[k][:, :N], src_ap, AF.Square, src_bufs, [SQ[k]])
        mm(pb[nbank][:, :N], negones, sq[k][:, :N], True, True, [SQ[k], CONSTB], [PB[nbank]])
        act(sd[k][:, :N], pb[nbank][:, :N], AF.Ln, [PB[nbank], CONSTB], [SD[k]], bias=eps_t[:, :], scale=-1.0 / 128)
        act(sd[k][:, :N], sd[k][:, :N], AF.Exp, [SD[k]], [SD[k]], scale=-0.5)

    nxt = [load_w(blk) for blk in (8, 16, 0)]
    for c in range(8):
        (wgc, WGC), (wci, WCI), (wgb, WGB) = nxt
        if c < 7:
            nxt = [load_w(blk) for blk in (8 + c + 1, 16 + c + 1, c + 1)]
        if with_experts and c % 2 == 0:
            convert(eu_d, 0, EUB, c // 2)
        for pi, (t0, N, tls) in enumerate(pieces):
            k = pcount[0] % 2
            pcount[0] += 1
            A, Bk, C, NB = (0, 1, 2, 6) if k == 0 else (3, 4, 5, 7)
            hbufs = [HT[t] for t in tls]
            for (w_, W_, bank) in ((wgc, WGC, A), (wci, WCI, Bk)) + (((wgb, WGB, C),) if pi > 0 else ()):
                for kc in range(16):
                    mm(pb[bank][:, :N], w_[:, kc, :], hT[:, kc, t0:t0 + N], kc == 0, kc == 15, [W_] + hbufs, [PB[bank]])
            act(gcs[k][:, :N], pb[A][:, :N], AF.Copy, [PB[A]], [GCS[k]])
            if pi > 0:
                pk = 1 - k
                pN = pieces[pi - 1][1]
                vop("dve", "tensor_copy", [UT[pk]], [UT[k]], out=ut[k][:, 0:2], in_=ut[pk][:, pN:pN + 2])
            vop("dve", "tensor_tensor", [PB[Bk], GCS[k]], [UT[k]], out=ut[k][:, 2:2 + N], in0=pb[Bk][:, :N], in1=gcs[k][:, :N], op=ALU.mult)
            if pi == 0:
                continue
            vop("dve", "tensor_scalar", [UT[k], CONSTB], [YT[k]], out=yt[k][:, :N], in0=ut[k][:, 0:N],
                scalar1=cw[:, 3 * c:3 * c + 1], scalar2=None, op0=ALU.mult)
            for jj in (1, 2):
                vop("dve", "scalar_tensor_tensor", [UT[k], YT[k], CONSTB], [YT[k]], out=yt[k][:, :N], in0=ut[k][:, jj:jj + N],
                    scalar=cw[:, 3 * c + jj:3 * c + jj + 1], in1=yt[k][:, :N], op0=ALU.mult, op1=ALU.add)
            vop("dve", "tensor_tensor", [PB[C], YT[k]], [YT[k]], out=yt[k][:, :N], in0=pb[C][:, :N], in1=yt[k][:, :N], op=ALU.mult)
            rms_part(yt[k][:, :N], [YT[k]], k, N, NB)
            n = pi - 1
            vop("dve", "scalar_tensor_tensor", [YT[k], SD[k], CONSTB], [ACTB[4 * n + i] for i in range(4)],
                out=actT[:, 4 * n:4 * n + 4, c, :], in0=yt[k][:, :N].rearrange("p (a b) -> p a b", a=4),
                scalar=ong[:, c:c + 1], in1=sd[k][:, :N].rearrange("p (a b) -> p a b", a=4), op0=ALU.mult, op1=ALU.mult)
    if stage == 2:
        return finish(nc, pg, dbg, {"actT": (actT, BF16, [128, 16, 16, 128])}, alloc, cur, S2, dma)
    pg.barrier()

    cur[0] = S2
    qT = alloc("qT", [128, 2048], BF16)
    kT = alloc("kT", [128, 2064], BF16)
    vt = alloc("vt", [128, 17, 128], BF16)
    QT = [Buf("qT%d" % i) for i in range(4)]
    KT = [Buf("kT%d" % i) for i in range(5)]
    VT = [Buf("vt%d" % i) for i in range(5)]
    sq = [alloc("sqh%d" % i, [128, 512], BF16) for i in range(2)]
    sd = [alloc("sdh%d" % i, [128, 512], F32) for i in range(2)]
    SQ = [Buf("sqh%d" % i) for i in range(2)]
    SD = [Buf("sdh%d" % i) for i in range(2)]
    e32 = [alloc("e32_%d" % i, [128, 512], F32) for i in range(2)]
    lp = [alloc("lp%d" % i, [128, 512], BF16) for i in range(2)]
    rr = [alloc("rr%d" % i, [128, 512], BF16) for i in range(3)]
    wT = [alloc("wT%d" % i, [128, 512], BF16) for i in range(2)]
    E32 = [Buf("e32_%d" % i) for i in range(2)]
    LP = [Buf("lp%d" % i) for i in range(2)]
    RR = [Buf("rr%d" % i) for i in range(3)]
    WTB = [Buf("wT%d" % i) for i in range(2)]
    hcount = [0]
    zc = [0]
    oc = [0]

    wbig = alloc("wbig", [128, 16, 2048], BF16, at=R_A)
    WBIG = [Buf("wbig%d" % i) for i in range(16)]
    nxt = [load_w(blk) for blk in (24, 32, 40)]
    for h in range(8):
        (wq_, WQ_), (wk_, WK_), (wv_, WV_) = nxt
        if h < 7:
            nxt = [load_w(blk) for blk in (24 + h + 1, 32 + h + 1, 40 + h + 1)]
        if with_experts:
            if h < 4:
                convert(eu_d, 0, EUB, 4 + h)
            else:
                convert(ev_d, 1, EVB, h - 4)
        for (w_, W_, gain, dstT, DB, plist) in ((wq_, WQ_, gq, qT, QT, pieces[1:]), (wk_, WK_, gk, kT, KT, pieces)):
            for (t0, N, tls) in plist:
                k = hcount[0] % 2
                hcount[0] += 1
                P_, NB = (4, 6) if k == 0 else (5, 7)
                hbufs = [HT[t] for t in tls]
                for kc in range(16):
                    mm(pb[P_][:, :N], w_[:, kc, :], hT[:, kc, t0:t0 + N], kc == 0, kc == 15, [W_] + hbufs, [PB[P_]])
                rms_part(pb[P_][:, :N], [PB[P_]], k, N, NB)
                if dstT is qT:
                    d0 = t0 - 16
                    dbuf = DB[d0 // 512]
                else:
                    d0 = t0
                    dbuf = DB[0] if t0 == 0 else DB[1 + (t0 - 16) // 512]
                vop("dve", "scalar_tensor_tensor", [PB[P_], SD[k], CONSTB], [dbuf], out=dstT[:, d0:d0 + N], in0=pb[P_][:, :N],
                    scalar=gain[:, 0:1], in1=sd[k][:, :N], op0=ALU.mult, op1=ALU.mult)
        for gi, tl in enumerate([[0], [1, 2, 3, 4], [5, 6, 7, 8], [9, 10, 11, 12], [13, 14, 15, 16]]):
            k = hcount[0] % 2
            hcount[0] += 1
            P_ = 4 if k == 0 else 5
            bank = pb[P_][:, :].rearrange("p (a b) -> p a b", a=4)
            for ii, t in enumerate(tl):
                rows = 16 if t == 0 else 128
                t0 = 0 if t == 0 else 16 + 128 * (t - 1)
                for kc in range(16):
                    mm(bank[:rows, ii, :], hT[:, kc, t0:t0 + rows], wv_[:, kc, :], kc == 0, kc == 15, [WV_, HT[t]], [PB[P_]])
            rows = 16 if gi == 0 else 128
            act(vt[:rows, tl[0]:tl[0] + len(tl), :], bank[:rows, 0:len(tl), :], AF.Copy, [PB[P_]], [VT[gi]])
        if h == 7:
            for kc in range(16):
                dma("pool", wbig[:, kc, :], wout_d[:, kc, :], W=[WBIG[kc]] + (HT if kc == 0 else []))
        steps = []
        for g in range(4):
            klist = list(range(4 * g + 3, -1, -1)) + [-1]
            for si, a in enumerate(klist):
                steps.append((g, si, a, si == len(klist) - 1))

        def ctx_of(st, gi):
            g, si, a, last = st
            rows = 16 if last else 128
            kcol = 0 if last else 16 + 128 * a
            return dict(g=g, first=si == 0, last=last, rows=rows, k_ap=kT[:, kcol:kcol + rows],
                        KB=KT[0] if last else KT[1 + a // 4], vti=0 if last else 1 + a,
                        VB=VT[0] if last else VT[1 + a // 4], diag=(not last) and a >= 4 * g, jj=a - 4 * g,
                        q_ap=qT[:, 512 * g:512 * g + 512], gi=gi, zb=gi % 4, k=gi % 2)

        def S1(c):
            rows, zb, k = c["rows"], c["zb"], c["k"]
            mm(pb[zb][:rows, :], c["k_ap"], c["q_ap"], True, not c["diag"], [c["KB"], QT[c["g"]]], [PB[zb]])
            if c["diag"]:
                mm(pb[zb][:rows, :], ident, masks[:, c["jj"], :], False, True, [CONSTB], [PB[zb]])
            act(e32[k][:rows, :], pb[zb][:rows, :], AF.Exp, [PB[zb]], [E32[k]])
            act(lp[k][:rows, :], e32[k][:rows, :], AF.Ln, [E32[k]], [LP[k]], bias=1.0)
            if not c["last"]:
                ri = c["gi"] % 3
                if c["first"]:
                    vop("dve", "tensor_copy", [LP[k]], [RR[ri]], out=rr[ri][:, :], in_=lp[k][:, :])
                else:
                    rp = (c["gi"] - 1) % 3
                    vop("dve", "tensor_tensor", [LP[k], RR[rp]], [RR[ri]], out=rr[ri][:, :], in0=rr[rp][:, :], in1=lp[k][:, :], op=ALU.add)

        def S2(c):
            rows, zb, k = c["rows"], c["zb"], c["k"]
            mm(pb[zb][:rows, :], negtri[:rows, :rows], lp[k][:rows, :], False, c["first"], [CONSTB, LP[k]], [PB[zb]], skip=True)
            if not c["first"]:
                rp = (c["gi"] - 1) % 3
                mm(pb[zb][:rows, :], negones[:, :rows], rr[rp][:, :], False, True, [CONSTB, RR[rp]], [PB[zb]], skip=True)
            act(wT[k][:rows, :], pb[zb][:rows, :], AF.Exp, [PB[zb]], [WTB[k]])

        def S3(c):
            rows, k = c["rows"], c["k"]
            ob = 4 + c["g"] % 2
            mm(pb[ob][:, :], vt[:rows, c["vti"], :], wT[k][:rows, :], c["first"], c["last"], [c["VB"], WTB[k]], [PB[ob]])
            if c["last"]:
                g = c["g"]
                kk = hcount[0] % 2
                hcount[0] += 1
                NB = 6 if kk == 0 else 7
                rms_part(pb[ob][:, :], [PB[ob]], kk, 512, NB)
                vop("dve", "scalar_tensor_tensor", [PB[ob], SD[kk], CONSTB], [ACTB[4 * g + i] for i in range(4)],
                    out=actT[:, 4 * g:4 * g + 4, 8 + h, :], in0=pb[ob][:, :].rearrange("p (a b) -> p a b", a=4),
                    scalar=ong[:, 8 + h:9 + h], in1=sd[kk][:, :].rearrange("p (a b) -> p a b", a=4), op0=ALU.mult, op1=ALU.mult)

        cs = []
        for st in steps:
            cs.append(ctx_of(st, zc[0]))
            zc[0] += 1
        nst = len(cs)
        for t in range(nst + 2):
            if t < nst:
                S1(cs[t])
            if 1 <= t <= nst:
                S2(cs[t - 1])
            if t >= 2:
                S3(cs[t - 2])
    if stage == 3:
        return finish(nc, pg, dbg, {"actT": (actT, BF16, [128, 16, 16, 128])}, alloc, cur, cur[0], dma)
    pg.barrier()

    cur[0] = R_S
    xt = [alloc("xt4_%d" % i, [128, 2048], F32) for i in range(2)]
    XT = [Buf("xt4_%d" % i) for i in range(2)]
    xs = alloc("xs4", [128, 2048], BF16)
    XS = Buf("xs4")
    junk = alloc("junk4", [128, 2048], BF16)
    JUNK = Buf("junk4")
    gvec = alloc("gvec4", [128, 2048], F32)
    GVEC = Buf("gvec4")
    ss = [alloc("ss4_%d" % i, [128, 4], F32) for i in range(2)]
    SS = [Buf("ss4_%d" % i) for i in range(2)]
    dma("sp", gvec[:], gffn_d.partition_broadcast(128), W=[GVEC])
    for j in range(16):
        k = j % 2
        if with_experts and j % 4 == 0:
            convert(ev_d, 1, EVB, 4 + j // 4)
        dma("sp", xt[k][:, :], x_d[128 * j:128 * (j + 1), :], W=[XT[k]])
        for n in range(4):
            for kc in range(16):
                mm(pb[4 * k + n][:, :], actT[:, j, kc, :], wbig[:, kc, 512 * n:512 * (n + 1)], kc == 0, kc == 15, [ACTB[j], WBIG[kc]], [PB[4 * k + n]])
        for n in range(4):
            vop("dve", "tensor_tensor", [PB[4 * k + n], XT[k]], [XT[k]], out=xt[k][:, 512 * n:512 * (n + 1)], in0=pb[4 * k + n][:, :],
                in1=xt[k][:, 512 * n:512 * (n + 1)], op=ALU.add)
        dma("sp", out_d[128 * j:128 * (j + 1), :], xt[k][:, :], R=[XT[k]], W=[OUTB[j]])
        act(junk[:, :], xt[k][:, :], AF.Square, [XT[k]], [JUNK, SS[k]], accum_out=ss[k][:, 0:1])
        act(ss[k][:, 1:2], ss[k][:, 0:1], AF.Sqrt, [SS[k], CONSTB], [SS[k]], bias=eps_t[:, :], scale=1.0 / 2048)
        vop("dve", "reciprocal", [SS[k]], [RSTD2], out=rstd2[:, j:j + 1], in_=ss[k][:, 1:2])
        vop("dve", "scalar_tensor_tensor", [XT[k], RSTD2, GVEC], [XS], out=xs[:, :], in0=xt[k][:, :],
            scalar=rstd2[:, j:j + 1], in1=gvec[:, :], op0=ALU.mult, op1=ALU.mult)
        for half in range(2):
            bi = 4 * k + half
            bank = pb[bi][:, :].bitcast(BF16).rearrange("p (a b) -> p a b", a=8)
            for c in range(8):
                cc = half * 8 + c
                tr(bank[:, c, :], xs[:, cc * 128:(cc + 1) * 128], ident, [XS, CONSTB], [PB[bi]])
            dst = actT[:, j, half * 8:(half + 1) * 8, :]
            if half == 0:
                act(dst, bank[:, :, :], AF.Copy, [PB[bi]], [ACTB[j]])
            else:
                vop("dve", "tensor_copy", [PB[bi]], [ACTB[j]], out=dst, in_=bank[:, :, :])
    if stage == 4:
        return finish(nc, pg, dbg, {"actT": (actT, BF16, [128, 16, 16, 128])}, alloc, cur, cur[0], dma)
    pg.barrier()

    cur[0] = R_S
    for kc in range(16):
        dma("pool", wbig[:, kc, :], wq_d[:, kc, :], W=[WBIG[kc]])
    qry4 = [alloc("qry4_%d" % i, [128, 4, 128], F32) for i in range(2)]
    QRY = [Buf("qry4_%d" % i) for i in range(2)]

    def mkset(t):
        d = {}
        off = cur[0]
        d["cand"] = alloc("cand_%d" % t, [128, 8, 256], F32)
        d["oh"] = alloc("oh_%d" % t, [128, 128, 16], F32, at=off)
        d["CAND"] = Buf("cand_%d" % t)
        def a_(name, shape, dt):
            d[name] = alloc("%s_%d" % (name, t), shape, dt)
            d[name.upper()] = Buf("%s_%d" % (name, t))
        a_("scores", [128, 16, 128], F32)
        a_("sc2", [128, 128], F32)
        a_("stop", [128, 16, 16], F32)
        a_("itop", [128, 16, 16], U32)
        a_("itopf", [128, 16, 16], F32)
        a_("c2", [128, 256], F32)
        a_("best", [128, 8, 16], F32)
        a_("pos", [128, 8, 16], U32)
        a_("pi_", [128, 128], I32)
        a_("pj_", [128, 128], I32)
        a_("e0", [128, 128], F32)
        a_("e1", [128, 128], F32)
        a_("ex", [128, 128], F32)
        a_("sm", [128, 16], F32)
        return d

    sets = [mkset(0), mkset(1)]
    qc = [0]

    def tile5a(j, D):
        scores, SCB = D["scores"], D["SCORES"]
        cand, CAND, oh, OH = D["cand"], D["CAND"], D["oh"], D["CAND"]
        sc2, SC2 = D["sc2"], D["SC2"]
        stop_, STOP = D["stop"], D["STOP"]
        itop, ITOP = D["itop"], D["ITOP"]
        itopf, ITOPF = D["itopf"], D["ITOPF"]
        c2, C2 = D["c2"], D["C2"]
        best, BEST = D["best"], D["BEST"]
        pos, POS = D["pos"], D["POS"]
        pi_, pj_, PIJ = D["pi_"], D["pj_"], D["PI_"]
        pif, pjf = pi_[:, :].bitcast(F32), pj_[:, :].bitcast(F32)
        e0, e1, E01 = D["e0"], D["e1"], D["E0"]
        ex, EXB = D["ex"], D["EX"]
        sm, SM = D["sm"], D["SM"]
        for mg in range(4):
            k = qc[0] % 2
            qc[0] += 1
            Q_, S_ = (0, 2) if k == 0 else (1, 3)
            qbank = pb[Q_][:, :].rearrange("p (a b) -> p a b", a=4)
            sbank = pb[S_][:, :].rearrange("p (a b) -> p a b", a=4)
            for mi in range(4):
                m = 4 * mg + mi
                for kc in range(16):
                    mm(qbank[:, mi, :], wbig[:, kc, 128 * m:128 * (m + 1)], actT[:, j, kc, :], kc == 0, kc == 15, [WBIG[kc], ACTB[j]], [PB[Q_]])
            act(qry4[k][:, :, :], qbank, AF.Copy, [PB[Q_]], [QRY[k]])
            for mi in range(4):
                m = 4 * mg + mi
                p = m % 2
                mm(sbank[:, mi, :], qry4[k][:, mi, :], skT[:, 128 * p:128 * (p + 1)], True, True, [QRY[k], CONSTB], [PB[S_]])
            act(scores[:, 4 * mg:4 * mg + 4, :], sbank, AF.Copy, [PB[S_]], [SCB])
            yield
        for m in range(16):
            vop("dve", "max", [SCB], [STOP], out=stop_[:, m, 0:8], in_=scores[:, m, :])
            yield
            vop("dve", "max_index", [SCB, STOP], [ITOP], out=itop[:, m, 0:8], in_max=stop_[:, m, 0:8], in_values=scores[:, m, :])
            yield
            vop("dve", "match_replace", [SCB, STOP], [SC2], out=sc2[:, :], in_to_replace=stop_[:, m, 0:8], in_values=scores[:, m, :], imm_value=-1e30)
            yield
            vop("dve", "max", [SC2], [STOP], out=stop_[:, m, 8:16], in_=sc2[:, :])
            yield
            vop("dve", "max_index", [SC2, STOP], [ITOP], out=itop[:, m, 8:16], in_max=stop_[:, m, 8:16], in_values=sc2[:, :])
            yield
        vop("dve", "tensor_copy", [ITOP], [ITOPF], out=itopf[:, :, :], in_=itop[:, :, :])
        yield
        cand4 = cand[:, :, :].rearrange("p h (i j) -> p h i j", i=16)
        vop("dve", "tensor_tensor", [STOP], [CAND], out=cand4,
            in0=stop_[:, 0:16:2, :].unsqueeze(3).to_broadcast([128, 8, 16, 16]),
            in1=stop_[:, 1:16:2, :].unsqueeze(2).to_broadcast([128, 8, 16, 16]), op=ALU.add)
        yield
        for hh in range(8):
            vop("dve", "max", [CAND], [BEST], out=best[:, hh, 0:8], in_=cand[:, hh, :])
            yield
            vop("dve", "max_index", [CAND, BEST], [POS], out=pos[:, hh, 0:8], in_max=best[:, hh, 0:8], in_values=cand[:, hh, :])
            yield
            vop("dve", "match_replace", [CAND, BEST], [C2], out=c2[:, :], in_to_replace=best[:, hh, 0:8], in_values=cand[:, hh, :], imm_value=-1e30)
            yield
            vop("dve", "max", [C2], [BEST], out=best[:, hh, 8:16], in_=c2[:, :])
            yield
            vop("dve", "max_index", [C2, BEST], [POS], out=pos[:, hh, 8:16], in_max=best[:, hh, 8:16], in_values=c2[:, :])
            yield
        posi = pos[:, :, :].rearrange("p h k -> p (h k)").bitcast(I32)
        vop("dve", "tensor_single_scalar", [POS], [PIJ], out=pi_[:, :], in_=posi, scalar=4, op=ALU.arith_shift_right)
        yield
        vop("dve", "tensor_single_scalar", [POS], [PIJ], out=pj_[:, :], in_=posi, scalar=15, op=ALU.bitwise_and)
        yield
        vop("dve", "tensor_copy", [PIJ], [PIJ], out=pif, in_=pi_[:, :])
        yield
        vop("dve", "tensor_copy", [PIJ], [PIJ], out=pjf, in_=pj_[:, :])
        yield
        oh4 = oh[:, :, :].rearrange("p (h k) i -> p h k i", h=8)
        for (pf, par, eo) in ((pif, 0, e0), (pjf, 1, e1)):
            vop("dve", "tensor_tensor", [PIJ, CONSTB], [OH], out=oh[:, :, :],
                in0=iota16.unsqueeze(1).to_broadcast([128, 128, 16]), in1=pf.unsqueeze(2).to_broadcast([128, 128, 16]), op=ALU.is_equal)
            yield
            vop("dve", "tensor_tensor", [OH, ITOPF], [OH], out=oh4, in0=oh4,
                in1=itopf[:, par:16:2, :].unsqueeze(2).to_broadcast([128, 8, 16, 16]), op=ALU.mult)
            yield
            vop("dve", "tensor_reduce", [OH], [E01], out=eo[:, :], in_=oh[:, :, :], axis=AX.X, op=ALU.add)
            yield
        vop("dve", "scalar_tensor_tensor", [E01], [E01], out=e0[:, :], in0=e0[:, :], scalar=128.0, in1=e1[:, :], op0=ALU.mult, op1=ALU.add)
        yield
        vop("dve", "tensor_copy", [E01], [IDXB[j]], out=idx_all[:, j, :], in_=e0[:, :])
        yield
        ex3 = ex[:, :].rearrange("p (h k) -> p h k", h=8)
        vop("dve", "tensor_tensor", [BEST], [EXB], out=ex3, in0=best[:, :, :], in1=best[:, :, 0:1].to_broadcast([128, 8, 16]), op=ALU.subtract)
        yield
        act(ex[:, :], ex[:, :], AF.Exp, [EXB], [EXB])
        vop("dve", "tensor_reduce", [EXB], [SM], out=sm[:, 0:8], in_=ex3, axis=AX.X, op=ALU.add)
        yield
        vop("dve", "reciprocal", [SM], [SM], out=sm[:, 8:16], in_=sm[:, 0:8])
        yield
        vop("dve", "tensor_tensor", [EXB, SM], [GATB[j]], out=gates_all[:, j, :].rearrange("p (h k) -> p h k", h=8), in0=ex3,
            in1=sm[:, 8:16].unsqueeze(2).to_broadcast([128, 8, 16]), op=ALU.mult)
        yield

    for j0 in range(0, 16, 2):
        alive = [tile5a(j0, sets[0]), tile5a(j0 + 1, sets[1])]
        while alive:
            for gen in list(alive):
                try:
                    next(gen)
                except StopIteration:
                    alive.remove(gen)
    if stage == 5:
        return finish(nc, pg, dbg, {"idx_all": (idx_all, I32, [128, 16, 128]), "gates_all": (gates_all, F32, [128, 16, 128])}, alloc, cur, cur[0], dma)
    pg.barrier()

    cur[0] = R_A
    NUB = 13
    ub = [alloc("ub%d" % i, [128, 4096], BF16) for i in range(NUB)]
    UB = [Buf("ub%d" % i) for i in range(NUB)]
    h2t = [alloc("h2t%d" % i, [128, 2048], F32) for i in range(2)]
    H2T = [Buf("h2t%d" % i) for i in range(2)]
    tokn = alloc("tokn", [128, 2048], F32)
    TOKN = Buf("tokn")
    tokb = alloc("tokb", [128, 2048], BF16)
    TOKB = Buf("tokb")
    gvec = alloc("gvec5", [128, 2048], F32)
    GVEC = Buf("gvec5")
    junkr = [alloc("junk5_%d" % i, [128, 2048], BF16) for i in range(2)]
    JUNKR = [Buf("junk5_%d" % i) for i in range(2)]
    junk2r = [alloc("junk2_%d" % i, [128, 2048], BF16) for i in range(3)]
    JUNK2R = [Buf("junk2_%d" % i) for i in range(3)]
    jc = [0, 0]
    prod = [alloc("prod%d" % i, [128, 2048], BF16) for i in range(3)]
    PROD = [Buf("prod%d" % i) for i in range(3)]
    dots = alloc("dots", [128, 128], F32)
    DOTS = [Buf("dots%d" % i) for i in range(128)]
    coef = alloc("coef", [128, 128], F32)
    COEFG = [Buf("coef%d" % i) for i in range(32)]
    NDG = 6
    dg = [alloc("dg%d" % i, [128, 128], BF16) for i in range(NDG)]
    DG = [Buf("dg%d" % i) for i in range(NDG)]
    dma("sp", gvec[:], gffn_d.partition_broadcast(128), W=[GVEC])
    uc = [0]
    pc = [0]
    ec_flat = ec_d.rearrange("e t d -> e (t d)") if with_experts else None
    GS = 4

    def gather(j, s):
        i = uc[0] % NUB
        uc[0] += 1
        pg.dma("pool", lambda e: e.indirect_dma_start(out=ub[i][:, :], out_offset=None, in_=ec_flat,
                                                     in_offset=bass.IndirectOffsetOnAxis(ap=idx_all[:, j, s:s + 1], axis=0)),
               [IDXB[j]] + EUB + EVB, [UB[i]])
        return ub[i], UB[i]

    for j in range(16):
        k = j % 2
        banks = [4 * k + n for n in range(4)]
        dma("sp", h2t[k][:, :], out_d[128 * j:128 * (j + 1), :], R=[OUTB[j]], W=[H2T[k]])
        vop("dve", "scalar_tensor_tensor", [H2T[k], RSTD2, GVEC], [TOKN], out=tokn[:, :], in0=h2t[k][:, :],
            scalar=rstd2[:, j:j + 1], in1=gvec[:, :], op0=ALU.mult, op1=ALU.mult)
        vop("dve", "tensor_copy", [TOKN], [TOKB], out=tokb[:, :], in_=tokn[:, :])
        for grp in range(128 // GS):
            held = []
            for s in range(GS * grp, GS * grp + GS):
                u_, U_ = gather(j, s)
                held.append((s, u_, U_))
                if s % 3 == 0:
                    ji = jc[0] % 2
                    jc[0] += 1
                    vop("dve", "scalar_tensor_tensor", [U_, TOKN], [JUNKR[ji], DOTS[s]], out=junkr[ji][:, :], in0=u_[:, 0:2048], scalar=1.0, in1=tokn[:, :],
                        op0=ALU.mult, op1=ALU.mult, accum_out=dots[:, s:s + 1])
                else:
                    pi2 = pc[0] % 3
                    pc[0] += 1
                    vop("dve", "tensor_tensor", [U_, TOKB], [PROD[pi2]], out=prod[pi2][:, :], in0=u_[:, 0:2048], in1=tokb[:, :], op=ALU.mult)
                    ji2 = jc[1] % 3
                    jc[1] += 1
                    act(junk2r[ji2][:, :], prod[pi2][:, :], AF.Copy, [PROD[pi2]], [JUNK2R[ji2], DOTS[s]], accum_out=dots[:, s:s + 1])
            s0 = GS * grp
            act(coef[:, s0:s0 + GS], dots[:, s0:s0 + GS], AF.Gelu, [DOTS[s] for s in range(s0, s0 + GS)], [COEFG[grp]])
            vop("dve", "tensor_tensor", [COEFG[grp], GATB[j]], [COEFG[grp]], out=coef[:, s0:s0 + GS], in0=coef[:, s0:s0 + GS],
                in1=gates_all[:, j, s0:s0 + GS], op=ALU.mult)
            for (s, u_, U_) in held:
                di = s % NDG
                act(dg[di][:, :], ident, AF.Copy, [COEFG[grp], CONSTB], [DG[di]], scale=coef[:, s:s + 1])
                for n in range(4):
                    mm(pb[banks[n]][:, :], dg[di][:, :], u_[:, 2048 + 512 * n:2048 + 512 * (n + 1)], s == 0, s == 127, [DG[di], U_], [PB[banks[n]]])
        for n in range(4):
            vop("dve", "tensor_tensor", [H2T[k], PB[banks[n]]], [H2T[k]], out=h2t[k][:, 512 * n:512 * (n + 1)],
                in0=pb[banks[n]][:, :], in1=h2t[k][:, 512 * n:512 * (n + 1)], op=ALU.add)
        dma("sp", out_d[128 * j:128 * (j + 1), :], h2t[k][:, :], R=[H2T[k]], W=[OUTB[j]])
    return finish(nc, pg, dbg, {}, alloc, cur, cur[0], dma)


def finish(nc, pg, dbg, dumps, alloc, cur, scratch_at, dma):
    pg.barrier()
    for name, (t, dt, shape) in dumps.items():
        d = nc.dram_tensor("dbg_" + name, list(shape), dt, kind="ExternalOutput").ap()
        dma("sp", d, t[:], [], [])
        dbg[name] = True
    pg.barrier()
    pg.emit_all()
    return nc, dbg


def _consts():
    ident = np.eye(128, dtype=np.float32)
    j = np.arange(128)[:, None]
    s = np.arange(128)[None, :]
    negtri = np.where(j >= s, -1.0, 0.0).astype(np.float32)
    negones = -np.ones((128, 128), np.float32)
    q = np.arange(512)[None, None, :]
    jj = np.arange(4)[None, :, None]
    ss = np.arange(128)[:, None, None]
    masks = np.where(q > ss + 128 * jj, 0.0, NEG).astype(np.float32).reshape(128, 2048)
    cbf = np.concatenate([ident, negtri, negones, masks], axis=1).astype(ml_dtypes.bfloat16)
    cf32 = np.concatenate([np.ones((128, 128), np.float32), np.tile(np.arange(16, dtype=np.float32), (128, 1))], axis=1)
    return cbf, cf32


def _prep_shared(meta_tokens, norm_mix_g, w_in, conv_w, q_norm_g, k_norm_g, out_norm_g, w_out, norm_ffn_g, w_query, sub_keys):
    f = lambda a: np.ascontiguousarray(np.asarray(a, dtype=np.float32))
    cbf, cf32 = _consts()
    d = {
        "meta": f(meta_tokens),
        "gmix": f(norm_mix_g[0:1]),
        "gffn": f(norm_ffn_g[0:1]),
        "w_in_b": f(np.asarray(w_in[0]).reshape(16, 128, 48, 128).transpose(2, 1, 0, 3)),
        "cw": f(np.asarray(conv_w[0]).reshape(3, 8, 128).transpose(2, 1, 0).reshape(128, 24)),
        "gq": f(np.asarray(q_norm_g[0]).reshape(128, 1)),
        "gk": f(np.asarray(k_norm_g[0]).reshape(128, 1)),
        "ong": f(np.asarray(out_norm_g[0]).reshape(16, 128).T),
        "w_out_b": f(np.asarray(w_out[0]).reshape(16, 128, 2048).transpose(1, 0, 2)),
        "w_query_b": f(np.asarray(w_query[0]).reshape(16, 128, 2048).transpose(1, 0, 2)),
        "skT": f(np.asarray(sub_keys[0]).transpose(2, 0, 1).reshape(128, 256)),
        "cbf": cbf,
        "cf32": cf32,
    }
    return d


def kernel(x, meta_tokens, norm_mix_g, w_in, conv_w, q_norm_g, k_norm_g, out_norm_g, w_out,
           norm_ffn_g, w_query, sub_keys, expert_u, expert_v):
    nc, _ = build(99, True)
    shared = _prep_shared(meta_tokens, norm_mix_g, w_in, conv_w, q_norm_g, k_norm_g, out_norm_g, w_out, norm_ffn_g, w_query, sub_keys)
    shared["expert_u"] = np.ascontiguousarray(np.asarray(expert_u[0], dtype=np.float32))
    shared["expert_v"] = np.ascontiguousarray(np.asarray(expert_v[0], dtype=np.float32))
    x = np.asarray(x, dtype=np.float32)
    in_maps = []
    for b in range(8):
        m = dict(shared)
        m["x"] = np.ascontiguousarray(x[b])
        in_maps.append(m)
    res = run_bass_kernel_spmd(nc, in_maps, core_ids=list(range(8)))
    return np.stack([np.asarray(r["out"], dtype=np.float32) for r in res.results], axis=0)
```

```python
import numpy as np
import ml_dtypes
import concourse.bass as bass
import concourse.mybir as mybir
from concourse.bass_utils import run_bass_kernel_spmd

F32 = mybir.dt.float32
BF16 = mybir.dt.bfloat16
I32 = mybir.dt.int32
U32 = mybir.dt.uint32
AF = mybir.ActivationFunctionType
ALU = mybir.AluOpType
AX = mybir.AxisListType

EPOCH = 16000
N_DMA_SEMS = {"sp": 12, "pool": 28, "act": 4, "pe": 1, "dve": 1}
EPS = 1e-6
NEG = -30000.0
SB_BASE = 16640
SB_END = 229376


class Buf:
    __slots__ = ("name", "w", "r")

    def __init__(self, name):
        self.name = name
        self.w = None
        self.r = {}


class Prog:
    ENGS = ("pe", "act", "dve", "pool", "sp")

    def __init__(self, nc):
        self.nc = nc
        self.ops = {e: [] for e in self.ENGS}
        self.seq = {e: 0 for e in self.ENGS}
        self.known = {e: {} for e in self.ENGS}
        self.sems = {}
        self.dma_rr = {e: 0 for e in self.ENGS}
        self.dma_cnt = {}
        self._sem_ctx = []

    def _sem(self, name):
        if name not in self.sems:
            ctx = self.nc.semaphore(name)
            h = ctx.__enter__()
            self._sem_ctx.append(ctx)
            self.sems[name] = h
        return self.sems[name]

    def _need(self, eng, ev, waits):
        if ev is None:
            return
        key, val = ev
        if self.known[eng].get(key, 0) >= val:
            return
        self.known[eng][key] = val
        waits[key] = max(waits.get(key, 0), val)

    def _deps(self, eng, reads, writes):
        waits = {}
        for b in reads:
            self._need(eng, b.w, waits)
        own = "c_pe_" if eng == "pe" else "#"
        for b in writes:
            if b.w is not None and not b.w[0].startswith(own):
                self._need(eng, b.w, waits)
            for ev in b.r.values():
                if not ev[0].startswith(own):
                    self._need(eng, ev, waits)
        return waits

    def _commit(self, ev, reads, writes):
        k = ev[0]
        for b in reads:
            old = b.r.get(k)
            if old is None or old[1] < ev[1]:
                b.r[k] = ev
        for b in writes:
            b.w = ev
            b.r = {}

    def op(self, eng, emit, reads=(), writes=()):
        waits = self._deps(eng, reads, writes)
        s = self.seq[eng]
        self.seq[eng] = s + 1
        key = "c_%s_%d" % (eng, s // EPOCH)
        ev = (key, s % EPOCH + 1)
        self._sem(key)
        self.ops[eng].append((waits, emit, (key, 1)))
        self._commit(ev, reads, writes)
        return ev

    def dma(self, eng, emit, reads=(), writes=()):
        i = self.dma_rr[eng]
        self.dma_rr[eng] = (i + 1) % N_DMA_SEMS[eng]
        key = "d_%s_%d" % (eng, i)
        self._sem(key)
        cnt = self.dma_cnt.get(key, 0)
        waits = self._deps(eng, reads, writes)
        if cnt > 0:
            self._need(eng, (key, 16 * cnt), waits)
        self.dma_cnt[key] = cnt + 1
        ev = (key, 16 * (cnt + 1))
        self.ops[eng].append((waits, emit, (key, 16)))
        self._commit(ev, reads, writes)
        return ev

    def barrier(self):
        last = {}
        for e in self.ENGS:
            s = self.seq[e]
            if s > 0:
                last["c_%s_%d" % (e, (s - 1) // EPOCH)] = (s - 1) % EPOCH + 1
        for key, cnt in self.dma_cnt.items():
            last[key] = 16 * cnt
        for e in self.ENGS:
            waits = {}
            for key, val in last.items():
                self._need(e, (key, val), waits)
            if waits:
                self.ops[e].append((waits, None, None))

    def emit_all(self):
        nc = self.nc
        with nc.Block() as block:
            def run(engname):
                def body(e):
                    for waits, emit, inc in self.ops[engname]:
                        for key, val in waits.items():
                            e.wait_ge(self.sems[key], val)
                        if emit is not None:
                            emit(e).then_inc(self.sems[inc[0]], inc[1])
                return body
            block.tensor(run("pe"))
            block.scalar(run("act"))
            block.vector(run("dve"))
            block.gpsimd(run("pool"))
            block.sync(run("sp"))


def build(stage=99, with_experts=True):
    nc = bass.Bass("TRN2", target_bir_lowering=False)
    pg = Prog(nc)
    dbg = {}

    def din(name, shape, dt=F32):
        return nc.dram_tensor(name, list(shape), dt, kind="ExternalInput").ap()

    x_d = din("x", [2048, 2048])
    meta_d = din("meta", [16, 2048])
    gmix_d = din("gmix", [1, 2048])
    gffn_d = din("gffn", [1, 2048])
    win_d = din("w_in_b", [48, 128, 16, 128])
    cw_d = din("cw", [128, 24])
    gq_d = din("gq", [128, 1])
    gk_d = din("gk", [128, 1])
    ong_d = din("ong", [128, 16])
    wout_d = din("w_out_b", [128, 16, 2048])
    wq_d = din("w_query_b", [128, 16, 2048])
    skT_d = din("skT", [128, 256])
    cbf_d = din("cbf", [128, 384 + 2048], BF16)
    cf32_d = din("cf32", [128, 128 + 16])
    if with_experts:
        eu_d = din("expert_u", [16384, 2048])
        ev_d = din("expert_v", [16384, 2048])
    out_d = nc.dram_tensor("out", [2048, 2048], F32, kind="ExternalOutput").ap()
    if with_experts:
        ec_d = nc.dram_tensor("euv_bf", [16384, 2, 2048], BF16, kind="Internal").ap()
    EUB = [Buf("eub%d" % i) for i in range(8)]
    EVB = [Buf("evb%d" % i) for i in range(8)]

    def convert(src, which, bufs, c):
        if with_experts:
            pg.dma("pool", lambda e: e.dma_start(out=ec_d[2048 * c:2048 * (c + 1), which, :], in_=src[2048 * c:2048 * (c + 1), :]), [], [bufs[c]])

    cur = [SB_BASE]

    def alloc(name, shape, dt, at=None):
        esz = 2 if dt == BF16 else 4
        nbytes = int(np.prod(shape[1:])) * esz
        off = cur[0] if at is None else at
        t = nc.alloc_sbuf_tensor_at(name, list(shape), dt, offset=off)
        end = off + (nbytes + 31) // 32 * 32
        assert end <= SB_END, (name, end)
        if at is None:
            cur[0] = end
        return t

    cbf = alloc("cbf", [128, 384 + 2048], BF16)
    cf32 = alloc("cf32", [128, 144], F32)
    eps_t = alloc("eps_t", [128, 1], F32)
    cw = alloc("cw_t", [128, 24], F32)
    gq = alloc("gq_t", [128, 1], F32)
    gk = alloc("gk_t", [128, 1], F32)
    ong = alloc("ong_t", [128, 16], F32)
    skT = alloc("skT_t", [128, 256], F32)
    rstd2 = alloc("rstd2", [128, 16], F32)
    idx_all = alloc("idx_all", [128, 16, 128], I32)
    gates_all = alloc("gates_all", [128, 16, 128], F32)
    ident = cbf[:, 0:128]
    negtri = cbf[:, 128:256]
    negones = cbf[:, 256:384]
    masks = cbf[:, 384:384 + 2048].rearrange("p (j q) -> p j q", j=4)
    ones32 = cf32[:, 0:128]
    iota16 = cf32[:, 128:144]
    R_A = cur[0]
    hT = alloc("hT", [128, 16, 2064], BF16)
    R_B = cur[0]
    actT = alloc("actT", [128, 16, 16, 128], BF16)
    R_S = cur[0]
    CONSTB = Buf("const")
    HT = [Buf("hT%d" % i) for i in range(17)]
    ACTB = [Buf("actT%d" % j) for j in range(16)]
    OUTB = [Buf("out%d" % j) for j in range(16)]
    RSTD2 = Buf("rstd2")
    IDXB = [Buf("idx%d" % j) for j in range(16)]
    GATB = [Buf("gat%d" % j) for j in range(16)]

    pb = []
    for i in range(8):
        ctx = nc.psum_tensor("pb%d" % i, [128, 512], F32)
        pb.append(ctx.__enter__())
    PB = [Buf("pb%d" % i) for i in range(8)]

    def dma(eng, out, in_, R=(), W=()):
        return pg.dma(eng, lambda e: e.dma_start(out=out, in_=in_), R, W)

    def mm(out, lhsT, rhs, start, stop, R, W, skip=False):
        return pg.op("pe", lambda e: e.matmul(out, lhsT=lhsT, rhs=rhs, start=start, stop=stop, skip_group_check=skip), R, W)

    def tr(out, in_, idn, R, W):
        return pg.op("pe", lambda e: e.transpose(out=out, in_=in_, identity=idn), R, W)

    def act(out, in_, func, R, W, **kw):
        return pg.op("act", lambda e: e.activation(out=out, in_=in_, func=func, **kw), R, W)

    def vop(eng, name, R, W, **kw):
        return pg.op(eng, lambda e: getattr(e, name)(**kw), R, W)

    dma("sp", cbf[:], cbf_d, W=[CONSTB])
    dma("sp", cf32[:], cf32_d, W=[CONSTB])
    dma("sp", cw[:], cw_d, W=[CONSTB])
    dma("sp", gq[:], gq_d, W=[CONSTB])
    dma("sp", gk[:], gk_d, W=[CONSTB])
    dma("sp", ong[:], ong_d, W=[CONSTB])
    dma("sp", skT[:], skT_d, W=[CONSTB])
    vop("dve", "memset", [], [CONSTB], ap=eps_t[:], constant=EPS)
    pg.barrier()
    vop("dve", "tensor_scalar", [CONSTB], [CONSTB], out=gq[:], in0=gq[:], scalar1=float(128 ** -0.5), scalar2=None, op0=ALU.mult)
    pg.barrier()

    cur[0] = R_S
    xt = [alloc("xt%d" % i, [128, 2048], F32) for i in range(2)]
    XT = [Buf("xt%d" % i) for i in range(2)]
    xs2 = [alloc("xs_%d" % i, [128, 2048], BF16) for i in range(2)]
    XS2 = [Buf("xs_%d" % i) for i in range(2)]
    junk = alloc("junk", [128, 2048], BF16)
    JUNK = Buf("junk")
    gvec = alloc("gvec", [128, 2048], F32)
    GVEC = Buf("gvec")
    ss = [alloc("ss%d" % i, [128, 4], F32) for i in range(2)]
    SS = [Buf("ss%d" % i) for i in range(2)]

    dma("sp", gvec[:], gmix_d.partition_broadcast(128), W=[GVEC])
    tiles = [(0, 16, 0, meta_d)] + [(1 + j, 128, 16 + 128 * j, x_d[128 * j:128 * (j + 1), :]) for j in range(16)]
    for ti, rows, t0, src in tiles:
        k = ti % 2
        dma("sp", xt[k][:rows, :], src, W=[XT[k]])
        act(junk[:rows, :], xt[k][:rows, :], AF.Square, [XT[k]], [JUNK, SS[k]], accum_out=ss[k][:rows, 0:1])
        act(ss[k][:rows, 1:2], ss[k][:rows, 0:1], AF.Sqrt, [SS[k], CONSTB], [SS[k]], bias=eps_t[:rows, :], scale=1.0 / 2048)
        vop("dve", "reciprocal", [SS[k]], [SS[k]], out=ss[k][:rows, 2:3], in_=ss[k][:rows, 1:2])
        xs, XS = xs2[k], XS2[k]
        vop("dve", "scalar_tensor_tensor", [XT[k], SS[k], GVEC], [XS], out=xs[:rows, :], in0=xt[k][:rows, :],
            scalar=ss[k][:rows, 2:3], in1=gvec[:rows, :], op0=ALU.mult, op1=ALU.mult)
        for half in range(2):
            bi = 2 * k + half
            bank = pb[bi][:, :].bitcast(BF16).rearrange("p (a b) -> p a b", a=8)
            for c in range(8):
                cc = half * 8 + c
                tr(bank[:, c, :rows], xs[:rows, cc * 128:(cc + 1) * 128], ident[:rows, :rows], [XS, CONSTB], [PB[bi]])
            dst = hT[:, half * 8:(half + 1) * 8, t0:t0 + rows]
            if half == 0:
                act(dst, bank[:, :, :rows], AF.Copy, [PB[bi]], [HT[ti]])
            else:
                vop("dve", "tensor_copy", [PB[bi]], [HT[ti]], out=dst, in_=bank[:, :, :rows])
    if stage == 1:
        return finish(nc, pg, dbg, {"hT": (hT, BF16, [128, 16, 2064])}, alloc, cur, R_S, dma)
    pg.barrier()

    cur[0] = R_S
    NWB = 6
    wb = [alloc("wb%d" % i, [128, 16, 128], BF16) for i in range(NWB)]
    WB = [Buf("wb%d" % i) for i in range(NWB)]
    wcnt = [0]

    def load_w(blk):
        i = wcnt[0] % NWB
        wcnt[0] += 1
        dma("pool", wb[i][:], win_d[blk], W=[WB[i]])
        return wb[i], WB[i]

    S2 = cur[0]
    gcs = [alloc("gcs%d" % i, [128, 512], F32) for i in range(2)]
    ut = [alloc("ut%d" % i, [128, 516], F32) for i in range(2)]
    yt = [alloc("yt%d" % i, [128, 512], F32) for i in range(2)]
    sq = [alloc("sq%d" % i, [128, 512], BF16) for i in range(2)]
    sd = [alloc("sd%d" % i, [128, 512], F32) for i in range(2)]
    GCS = [Buf("gcs%d" % i) for i in range(2)]
    UT = [Buf("ut%d" % i) for i in range(2)]
    YT = [Buf("yt%d" % i) for i in range(2)]
    SQ = [Buf("sq%d" % i) for i in range(2)]
    SD = [Buf("sd%d" % i) for i in range(2)]

    pieces = [(0, 16, [0])] + [(16 + 512 * n, 512, [1 + 4 * n + i for i in range(4)]) for n in range(4)]
    pcount = [0]

    def rms_part(src_ap, src_bufs, k, N, nbank):
        act(sq[k][:, :N], src_ap, AF.Square, src_bufs, [SQ[k]])
        mm(pb[nbank][:, :N], negones, sq[k][:, :N], True, True, [SQ[k], CONSTB], [PB[nbank]])
        act(sd[k][:, :N], pb[nbank][:, :N], AF.Ln, [PB[nbank], CONSTB], [SD[k]], bias=eps_t[:, :], scale=-1.0 / 128)
        act(sd[k][:, :N], sd[k][:, :N], AF.Exp, [SD[k]], [SD[k]], scale=-0.5)

    nxt = [load_w(blk) for blk in (8, 16, 0)]
    for c in range(8):
        (wgc, WGC), (wci, WCI), (wgb, WGB) = nxt
        if c < 7:
            nxt = [load_w(blk) for blk in (8 + c + 1, 16 + c + 1, c + 1)]
        if with_experts and c % 2 == 0:
            convert(eu_d, 0, EUB, c // 2)
        for pi, (t0, N, tls) in enumerate(pieces):
            k = pcount[0] % 2
            pcount[0] += 1
            A, Bk, C, NB = (0, 1, 2, 6) if k == 0 else (3, 4, 5, 7)
            hbufs = [HT[t] for t in tls]
            for (w_, W_, bank) in ((wgc, WGC, A), (wci, WCI, Bk)) + (((wgb, WGB, C),) if pi > 0 else ()):
                for kc in range(16):
                    mm(pb[bank][:, :N], w_[:, kc, :], hT[:, kc, t0:t0 + N], kc == 0, kc == 15, [W_] + hbufs, [PB[bank]])
            act(gcs[k][:, :N], pb[A][:, :N], AF.Copy, [PB[A]], [GCS[k]])
            if pi > 0:
                pk = 1 - k
                pN = pieces[pi - 1][1]
                vop("dve", "tensor_copy", [UT[pk]], [UT[k]], out=ut[k][:, 0:2], in_=ut[pk][:, pN:pN + 2])
            vop("dve", "tensor_tensor", [PB[Bk], GCS[k]], [UT[k]], out=ut[k][:, 2:2 + N], in0=pb[Bk][:, :N], in1=gcs[k][:, :N], op=ALU.mult)
            if pi == 0:
                continue
            vop("dve", "tensor_scalar", [UT[k], CONSTB], [YT[k]], out=yt[k][:, :N], in0=ut[k][:, 0:N],
                scalar1=cw[:, 3 * c:3 * c + 1], scalar2=None, op0=ALU.mult)
            for jj in (1, 2):
                vop("dve", "scalar_tensor_tensor", [UT[k], YT[k], CONSTB], [YT[k]], out=yt[k][:, :N], in0=ut[k][:, jj:jj + N],
                    scalar=cw[:, 3 * c + jj:3 * c + jj + 1], in1=yt[k][:, :N], op0=ALU.mult, op1=ALU.add)
            vop("dve", "tensor_tensor", [PB[C], YT[k]], [YT[k]], out=yt[k][:, :N], in0=pb[C][:, :N], in1=yt[k][:, :N], op=ALU.mult)
            rms_part(yt[k][:, :N], [YT[k]], k, N, NB)
            n = pi - 1
            vop("dve", "scalar_tensor_tensor", [YT[k], SD[k], CONSTB], [ACTB[4 * n + i] for i in range(4)],
                out=actT[:, 4 * n:4 * n + 4, c, :], in0=yt[k][:, :N].rearrange("p (a b) -> p a b", a=4),
                scalar=ong[:, c:c + 1], in1=sd[k][:, :N].rearrange("p (a b) -> p a b", a=4), op0=ALU.mult, op1=ALU.mult)
    if stage == 2:
        return finish(nc, pg, dbg, {"actT": (actT, BF16, [128, 16, 16, 128])}, alloc, cur, S2, dma)
    pg.barrier()

    cur[0] = S2
    qT = alloc("qT", [128, 2048], BF16)
    kT = alloc("kT", [128, 2064], BF16)
    vt = alloc("vt", [128, 17, 128], BF16)
    QT = [Buf("qT%d" % i) for i in range(4)]
    KT = [Buf("kT%d" % i) for i in range(5)]
    VT = [Buf("vt%d" % i) for i in range(5)]
    sq = [alloc("sqh%d" % i, [128, 512], BF16) for i in range(2)]
    sd = [alloc("sdh%d" % i, [128, 512], F32) for i in range(2)]
    SQ = [Buf("sqh%d" % i) for i in range(2)]
    SD = [Buf("sdh%d" % i) for i in range(2)]
    e32 = [alloc("e32_%d" % i, [128, 512], F32) for i in range(2)]
    lp = [alloc("lp%d" % i, [128, 512], BF16) for i in range(2)]
    rr = [alloc("rr%d" % i, [128, 512], BF16) for i in range(3)]
    wT = [alloc("wT%d" % i, [128, 512], BF16) for i in range(2)]
    E32 = [Buf("e32_%d" % i) for i in range(2)]
    LP = [Buf("lp%d" % i) for i in range(2)]
    RR = [Buf("rr%d" % i) for i in range(3)]
    WTB = [Buf("wT%d" % i) for i in range(2)]
    hcount = [0]
    zc = [0]
    oc = [0]

    wbig = alloc("wbig", [128, 16, 2048], BF16, at=R_A)
    WBIG = [Buf("wbig%d" % i) for i in range(16)]
    nxt = [load_w(blk) for blk in (24, 32, 40)]
    for h in range(8):
        (wq_, WQ_), (wk_, WK_), (wv_, WV_) = nxt
        if h < 7:
            nxt = [load_w(blk) for blk in (24 + h + 1, 32 + h + 1, 40 + h + 1)]
        if with_experts:
            if h < 4:
                convert(eu_d, 0, EUB, 4 + h)
            convert(ev_d, 1, EVB, h)
        for (w_, W_, gain, dstT, DB, plist) in ((wq_, WQ_, gq, qT, QT, pieces[1:]), (wk_, WK_, gk, kT, KT, pieces)):
            for (t0, N, tls) in plist:
                k = hcount[0] % 2
                hcount[0] += 1
                P_, NB = (4, 6) if k == 0 else (5, 7)
                hbufs = [HT[t] for t in tls]
                for kc in range(16):
                    mm(pb[P_][:, :N], w_[:, kc, :], hT[:, kc, t0:t0 + N], kc == 0, kc == 15, [W_] + hbufs, [PB[P_]])
                rms_part(pb[P_][:, :N], [PB[P_]], k, N, NB)
                if dstT is qT:
                    d0 = t0 - 16
                    dbuf = DB[d0 // 512]
                else:
                    d0 = t0
                    dbuf = DB[0] if t0 == 0 else DB[1 + (t0 - 16) // 512]
                vop("dve", "scalar_tensor_tensor", [PB[P_], SD[k], CONSTB], [dbuf], out=dstT[:, d0:d0 + N], in0=pb[P_][:, :N],
                    scalar=gain[:, 0:1], in1=sd[k][:, :N], op0=ALU.mult, op1=ALU.mult)
        for gi, tl in enumerate([[0], [1, 2, 3, 4], [5, 6, 7, 8], [9, 10, 11, 12], [13, 14, 15, 16]]):
            k = hcount[0] % 2
            hcount[0] += 1
            P_ = 4 if k == 0 else 5
            bank = pb[P_][:, :].rearrange("p (a b) -> p a b", a=4)
            for ii, t in enumerate(tl):
                rows = 16 if t == 0 else 128
                t0 = 0 if t == 0 else 16 + 128 * (t - 1)
                for kc in range(16):
                    mm(bank[:rows, ii, :], hT[:, kc, t0:t0 + rows], wv_[:, kc, :], kc == 0, kc == 15, [WV_, HT[t]], [PB[P_]])
            rows = 16 if gi == 0 else 128
            act(vt[:rows, tl[0]:tl[0] + len(tl), :], bank[:rows, 0:len(tl), :], AF.Copy, [PB[P_]], [VT[gi]])
        if h == 7:
            for kc in range(16):
                dma("pool", wbig[:, kc, :], wout_d[:, kc, :], W=[WBIG[kc]] + (HT if kc == 0 else []))
        steps = []
        for g in range(4):
            klist = list(range(4 * g + 3, -1, -1)) + [-1]
            for si, a in enumerate(klist):
                steps.append((g, si, a, si == len(klist) - 1))

        def ctx_of(st, gi):
            g, si, a, last = st
            rows = 16 if last else 128
            kcol = 0 if last else 16 + 128 * a
            return dict(g=g, first=si == 0, last=last, rows=rows, k_ap=kT[:, kcol:kcol + rows],
                        KB=KT[0] if last else KT[1 + a // 4], vti=0 if last else 1 + a,
                        VB=VT[0] if last else VT[1 + a // 4], diag=(not last) and a >= 4 * g, jj=a - 4 * g,
                        q_ap=qT[:, 512 * g:512 * g + 512], gi=gi, zb=gi % 4, k=gi % 2)

        def S1(c):
            rows, zb, k = c["rows"], c["zb"], c["k"]
            mm(pb[zb][:rows, :], c["k_ap"], c["q_ap"], True, not c["diag"], [c["KB"], QT[c["g"]]], [PB[zb]])
            if c["diag"]:
                mm(pb[zb][:rows, :], ident, masks[:, c["jj"], :], False, True, [CONSTB], [PB[zb]])
            act(e32[k][:rows, :], pb[zb][:rows, :], AF.Exp, [PB[zb]], [E32[k]])
            act(lp[k][:rows, :], e32[k][:rows, :], AF.Ln, [E32[k]], [LP[k]], bias=1.0)
            if not c["last"]:
                ri = c["gi"] % 3
                if c["first"]:
                    vop("dve", "tensor_copy", [LP[k]], [RR[ri]], out=rr[ri][:, :], in_=lp[k][:, :])
                else:
                    rp = (c["gi"] - 1) % 3
                    vop("dve", "tensor_tensor", [LP[k], RR[rp]], [RR[ri]], out=rr[ri][:, :], in0=rr[rp][:, :], in1=lp[k][:, :], op=ALU.add)

        def S2(c):
            rows, zb, k = c["rows"], c["zb"], c["k"]
            mm(pb[zb][:rows, :], negtri[:rows, :rows], lp[k][:rows, :], False, c["first"], [CONSTB, LP[k]], [PB[zb]], skip=True)
            if not c["first"]:
                rp = (c["gi"] - 1) % 3
                mm(pb[zb][:rows, :], negones[:, :rows], rr[rp][:, :], False, True, [CONSTB, RR[rp]], [PB[zb]], skip=True)
            act(wT[k][:rows, :], pb[zb][:rows, :], AF.Exp, [PB[zb]], [WTB[k]])

        def S3(c):
            rows, k = c["rows"], c["k"]
            ob = 4 + c["g"] % 2
            mm(pb[ob][:, :], vt[:rows, c["vti"], :], wT[k][:rows, :], c["first"], c["last"], [c["VB"], WTB[k]], [PB[ob]])
            if c["last"]:
                g = c["g"]
                kk = hcount[0] % 2
                hcount[0] += 1
                NB = 6 if kk == 0 else 7
                rms_part(pb[ob][:, :], [PB[ob]], kk, 512, NB)
                vop("dve", "scalar_tensor_tensor", [PB[ob], SD[kk], CONSTB], [ACTB[4 * g + i] for i in range(4)],
                    out=actT[:, 4 * g:4 * g + 4, 8 + h, :], in0=pb[ob][:, :].rearrange("p (a b) -> p a b", a=4),
                    scalar=ong[:, 8 + h:9 + h], in1=sd[kk][:, :].rearrange("p (a b) -> p a b", a=4), op0=ALU.mult, op1=ALU.mult)

        cs = []
        for st in steps:
            cs.append(ctx_of(st, zc[0]))
            zc[0] += 1
        nst = len(cs)
        for t in range(nst + 2):
            if t < nst:
                S1(cs[t])
            if 1 <= t <= nst:
                S2(cs[t - 1])
            if t >= 2:
                S3(cs[t - 2])
    if stage == 3:
        return finish(nc, pg, dbg, {"actT": (actT, BF16, [128, 16, 16, 128])}, alloc, cur, cur[0], dma)
    pg.barrier()

    cur[0] = R_S
    xt = [alloc("xt4_%d" % i, [128, 2048], F32) for i in range(2)]
    XT = [Buf("xt4_%d" % i) for i in range(2)]
    xs = alloc("xs4", [128, 2048], BF16)
    XS = Buf("xs4")
    junk = alloc("junk4", [128, 2048], BF16)
    JUNK = Buf("junk4")
    gvec = alloc("gvec4", [128, 2048], F32)
    GVEC = Buf("gvec4")
    ss = [alloc("ss4_%d" % i, [128, 4], F32) for i in range(2)]
    SS = [Buf("ss4_%d" % i) for i in range(2)]
    dma("sp", gvec[:], gffn_d.partition_broadcast(128), W=[GVEC])
    for j in range(16):
        k = j % 2
        dma("sp", xt[k][:, :], x_d[128 * j:128 * (j + 1), :], W=[XT[k]])
        for n in range(4):
            for kc in range(16):
                mm(pb[4 * k + n][:, :], actT[:, j, kc, :], wbig[:, kc, 512 * n:512 * (n + 1)], kc == 0, kc == 15, [ACTB[j], WBIG[kc]], [PB[4 * k + n]])
        for n in range(4):
            vop("dve", "tensor_tensor", [PB[4 * k + n], XT[k]], [XT[k]], out=xt[k][:, 512 * n:512 * (n + 1)], in0=pb[4 * k + n][:, :],
                in1=xt[k][:, 512 * n:512 * (n + 1)], op=ALU.add)
        dma("sp", out_d[128 * j:128 * (j + 1), :], xt[k][:, :], R=[XT[k]], W=[OUTB[j]])
        act(junk[:, :], xt[k][:, :], AF.Square, [XT[k]], [JUNK, SS[k]], accum_out=ss[k][:, 0:1])
        act(ss[k][:, 1:2], ss[k][:, 0:1], AF.Sqrt, [SS[k], CONSTB], [SS[k]], bias=eps_t[:, :], scale=1.0 / 2048)
        vop("dve", "reciprocal", [SS[k]], [RSTD2], out=rstd2[:, j:j + 1], in_=ss[k][:, 1:2])
        vop("dve", "scalar_tensor_tensor", [XT[k], RSTD2, GVEC], [XS], out=xs[:, :], in0=xt[k][:, :],
            scalar=rstd2[:, j:j + 1], in1=gvec[:, :], op0=ALU.mult, op1=ALU.mult)
        for half in range(2):
            bi = 4 * k + half
            bank = pb[bi][:, :].bitcast(BF16).rearrange("p (a b) -> p a b", a=8)
            for c in range(8):
                cc = half * 8 + c
                tr(bank[:, c, :], xs[:, cc * 128:(cc + 1) * 128], ident, [XS, CONSTB], [PB[bi]])
            dst = actT[:, j, half * 8:(half + 1) * 8, :]
            if half == 0:
                act(dst, bank[:, :, :], AF.Copy, [PB[bi]], [ACTB[j]])
            else:
                vop("dve", "tensor_copy", [PB[bi]], [ACTB[j]], out=dst, in_=bank[:, :, :])
    if stage == 4:
        return finish(nc, pg, dbg, {"actT": (actT, BF16, [128, 16, 16, 128])}, alloc, cur, cur[0], dma)
    pg.barrier()

    cur[0] = R_S
    for kc in range(16):
        dma("pool", wbig[:, kc, :], wq_d[:, kc, :], W=[WBIG[kc]])
    qry4 = [alloc("qry4_%d" % i, [128, 4, 128], F32) for i in range(2)]
    QRY = [Buf("qry4_%d" % i) for i in range(2)]

    def mkset(t):
        d = {}
        off = cur[0]
        d["cand"] = alloc("cand_%d" % t, [128, 8, 256], F32)
        d["oh"] = alloc("oh_%d" % t, [128, 128, 16], F32, at=off)
        d["CAND"] = Buf("cand_%d" % t)
        def a_(name, shape, dt):
            d[name] = alloc("%s_%d" % (name, t), shape, dt)
            d[name.upper()] = Buf("%s_%d" % (name, t))
        a_("scores", [128, 16, 128], F32)
        a_("sc2", [128, 128], F32)
        a_("stop", [128, 16, 16], F32)
        a_("itop", [128, 16, 16], U32)
        a_("itopf", [128, 16, 16], F32)
        a_("c2", [128, 256], F32)
        a_("best", [128, 8, 16], F32)
        a_("pos", [128, 8, 16], U32)
        a_("pi_", [128, 128], I32)
        a_("pj_", [128, 128], I32)
        a_("e0", [128, 128], F32)
        a_("e1", [128, 128], F32)
        a_("ex", [128, 128], F32)
        a_("sm", [128, 16], F32)
        return d

    sets = [mkset(0), mkset(1)]
    qc = [0]

    def tile5a(j, D):
        scores, SCB = D["scores"], D["SCORES"]
        cand, CAND, oh, OH = D["cand"], D["CAND"], D["oh"], D["CAND"]
        sc2, SC2 = D["sc2"], D["SC2"]
        stop_, STOP = D["stop"], D["STOP"]
        itop, ITOP = D["itop"], D["ITOP"]
        itopf, ITOPF = D["itopf"], D["ITOPF"]
        c2, C2 = D["c2"], D["C2"]
        best, BEST = D["best"], D["BEST"]
        pos, POS = D["pos"], D["POS"]
        pi_, pj_, PIJ = D["pi_"], D["pj_"], D["PI_"]
        pif, pjf = pi_[:, :].bitcast(F32), pj_[:, :].bitcast(F32)
        e0, e1, E01 = D["e0"], D["e1"], D["E0"]
        ex, EXB = D["ex"], D["EX"]
        sm, SM = D["sm"], D["SM"]
        for mg in range(4):
            k = qc[0] % 2
            qc[0] += 1
            Q_, S_ = (0, 2) if k == 0 else (1, 3)
            qbank = pb[Q_][:, :].rearrange("p (a b) -> p a b", a=4)
            sbank = pb[S_][:, :].rearrange("p (a b) -> p a b", a=4)
            for mi in range(4):
                m = 4 * mg + mi
                for kc in range(16):
                    mm(qbank[:, mi, :], wbig[:, kc, 128 * m:128 * (m + 1)], actT[:, j, kc, :], kc == 0, kc == 15, [WBIG[kc], ACTB[j]], [PB[Q_]])
            act(qry4[k][:, :, :], qbank, AF.Copy, [PB[Q_]], [QRY[k]])
            for mi in range(4):
                m = 4 * mg + mi
                p = m % 2
                mm(sbank[:, mi, :], qry4[k][:, mi, :], skT[:, 128 * p:128 * (p + 1)], True, True, [QRY[k], CONSTB], [PB[S_]])
            act(scores[:, 4 * mg:4 * mg + 4, :], sbank, AF.Copy, [PB[S_]], [SCB])
            yield
        for m in range(16):
            vop("dve", "max", [SCB], [STOP], out=stop_[:, m, 0:8], in_=scores[:, m, :])
            yield
            vop("dve", "max_index", [SCB, STOP], [ITOP], out=itop[:, m, 0:8], in_max=stop_[:, m, 0:8], in_values=scores[:, m, :])
            yield
            vop("dve", "match_replace", [SCB, STOP], [SC2], out=sc2[:, :], in_to_replace=stop_[:, m, 0:8], in_values=scores[:, m, :], imm_value=-1e30)
            yield
            vop("dve", "max", [SC2], [STOP], out=stop_[:, m, 8:16], in_=sc2[:, :])
            yield
            vop("dve", "max_index", [SC2, STOP], [ITOP], out=itop[:, m, 8:16], in_max=stop_[:, m, 8:16], in_values=sc2[:, :])
            yield
        vop("dve", "tensor_copy", [ITOP], [ITOPF], out=itopf[:, :, :], in_=itop[:, :, :])
        yield
        cand4 = cand[:, :, :].rearrange("p h (i j) -> p h i j", i=16)
        vop("dve", "tensor_tensor", [STOP], [CAND], out=cand4,
            in0=stop_[:, 0:16:2, :].unsqueeze(3).to_broadcast([128, 8, 16, 16]),
            in1=stop_[:, 1:16:2, :].unsqueeze(2).to_broadcast([128, 8, 16, 16]), op=ALU.add)
        yield
        for hh in range(8):
            vop("dve", "max", [CAND], [BEST], out=best[:, hh, 0:8], in_=cand[:, hh, :])
            yield
            vop("dve", "max_index", [CAND, BEST], [POS], out=pos[:, hh, 0:8], in_max=best[:, hh, 0:8], in_values=cand[:, hh, :])
            yield
            vop("dve", "match_replace", [CAND, BEST], [C2], out=c2[:, :], in_to_replace=best[:, hh, 0:8], in_values=cand[:, hh, :], imm_value=-1e30)
            yield
            vop("dve", "max", [C2], [BEST], out=best[:, hh, 8:16], in_=c2[:, :])
            yield
            vop("dve", "max_index", [C2, BEST], [POS], out=pos[:, hh, 8:16], in_max=best[:, hh, 8:16], in_values=c2[:, :])
            yield
        posi = pos[:, :, :].rearrange("p h k -> p (h k)").bitcast(I32)
        vop("dve", "tensor_single_scalar", [POS], [PIJ], out=pi_[:, :], in_=posi, scalar=4, op=ALU.arith_shift_right)
        yield
        vop("dve", "tensor_single_scalar", [POS], [PIJ], out=pj_[:, :], in_=posi, scalar=15, op=ALU.bitwise_and)
        yield
        vop("dve", "tensor_copy", [PIJ], [PIJ], out=pif, in_=pi_[:, :])
        yield
        vop("dve", "tensor_copy", [PIJ], [PIJ], out=pjf, in_=pj_[:, :])
        yield
        oh4 = oh[:, :, :].rearrange("p (h k) i -> p h k i", h=8)
        for (pf, par, eo) in ((pif, 0, e0), (pjf, 1, e1)):
            vop("dve", "tensor_tensor", [PIJ, CONSTB], [OH], out=oh[:, :, :],
                in0=iota16.unsqueeze(1).to_broadcast([128, 128, 16]), in1=pf.unsqueeze(2).to_broadcast([128, 128, 16]), op=ALU.is_equal)
            yield
            vop("dve", "tensor_tensor", [OH, ITOPF], [OH], out=oh4, in0=oh4,
                in1=itopf[:, par:16:2, :].unsqueeze(2).to_broadcast([128, 8, 16, 16]), op=ALU.mult)
            yield
            vop("dve", "tensor_reduce", [OH], [E01], out=eo[:, :], in_=oh[:, :, :], axis=AX.X, op=ALU.add)
            yield
        vop("dve", "scalar_tensor_tensor", [E01], [E01], out=e0[:, :], in0=e0[:, :], scalar=128.0, in1=e1[:, :], op0=ALU.mult, op1=ALU.add)
        yield
        vop("dve", "tensor_copy", [E01], [IDXB[j]], out=idx_all[:, j, :], in_=e0[:, :])
        yield
        ex3 = ex[:, :].rearrange("p (h k) -> p h k", h=8)
        vop("dve", "tensor_tensor", [BEST], [EXB], out=ex3, in0=best[:, :, :], in1=best[:, :, 0:1].to_broadcast([128, 8, 16]), op=ALU.subtract)
        yield
        act(ex[:, :], ex[:, :], AF.Exp, [EXB], [EXB])
        vop("dve", "tensor_reduce", [EXB], [SM], out=sm[:, 0:8], in_=ex3, axis=AX.X, op=ALU.add)
        yield
        vop("dve", "reciprocal", [SM], [SM], out=sm[:, 8:16], in_=sm[:, 0:8])
        yield
        vop("dve", "tensor_tensor", [EXB, SM], [GATB[j]], out=gates_all[:, j, :].rearrange("p (h k) -> p h k", h=8), in0=ex3,
            in1=sm[:, 8:16].unsqueeze(2).to_broadcast([128, 8, 16]), op=ALU.mult)
        yield

    for j0 in range(0, 16, 2):
        alive = [tile5a(j0, sets[0]), tile5a(j0 + 1, sets[1])]
        while alive:
            for gen in list(alive):
                try:
                    next(gen)
                except StopIteration:
                    alive.remove(gen)
    if stage == 5:
        return finish(nc, pg, dbg, {"idx_all": (idx_all, I32, [128, 16, 128]), "gates_all": (gates_all, F32, [128, 16, 128])}, alloc, cur, cur[0], dma)
    pg.barrier()

    cur[0] = R_A
    NUB = 13
    ub = [alloc("ub%d" % i, [128, 4096], BF16) for i in range(NUB)]
    UB = [Buf("ub%d" % i) for i in range(NUB)]
    h2t = [alloc("h2t%d" % i, [128, 2048], F32) for i in range(2)]
    H2T = [Buf("h2t%d" % i) for i in range(2)]
    tokn = alloc("tokn", [128, 2048], F32)
    TOKN = Buf("tokn")
    tokb = alloc("tokb", [128, 2048], BF16)
    TOKB = Buf("tokb")
    gvec = alloc("gvec5", [128, 2048], F32)
    GVEC = Buf("gvec5")
    junkr = [alloc("junk5_%d" % i, [128, 2048], BF16) for i in range(2)]
    JUNKR = [Buf("junk5_%d" % i) for i in range(2)]
    junk2r = [alloc("junk2_%d" % i, [128, 2048], BF16) for i in range(3)]
    JUNK2R = [Buf("junk2_%d" % i) for i in range(3)]
    jc = [0, 0]
    prod = [alloc("prod%d" % i, [128, 2048], BF16) for i in range(3)]
    PROD = [Buf("prod%d" % i) for i in range(3)]
    dots = alloc("dots", [128, 128], F32)
    DOTS = [Buf("dots%d" % i) for i in range(128)]
    coef = alloc("coef", [128, 128], F32)
    COEFG = [Buf("coef%d" % i) for i in range(32)]
    NDG = 6
    dg = [alloc("dg%d" % i, [128, 128], BF16) for i in range(NDG)]
    DG = [Buf("dg%d" % i) for i in range(NDG)]
    dma("sp", gvec[:], gffn_d.partition_broadcast(128), W=[GVEC])
    uc = [0]
    pc = [0]
    ec_flat = ec_d.rearrange("e t d -> e (t d)") if with_experts else None
    GS = 4

    def gather(j, s):
        i = uc[0] % NUB
        uc[0] += 1
        pg.dma("pool", lambda e: e.indirect_dma_start(out=ub[i][:, :], out_offset=None, in_=ec_flat,
                                                     in_offset=bass.IndirectOffsetOnAxis(ap=idx_all[:, j, s:s + 1], axis=0)),
               [IDXB[j]] + EUB + EVB, [UB[i]])
        return ub[i], UB[i]

    for j in range(16):
        k = j % 2
        banks = [4 * k + n for n in range(4)]
        dma("sp", h2t[k][:, :], out_d[128 * j:128 * (j + 1), :], R=[OUTB[j]], W=[H2T[k]])
        vop("dve", "scalar_tensor_tensor", [H2T[k], RSTD2, GVEC], [TOKN], out=tokn[:, :], in0=h2t[k][:, :],
            scalar=rstd2[:, j:j + 1], in1=gvec[:, :], op0=ALU.mult, op1=ALU.mult)
        vop("dve", "tensor_copy", [TOKN], [TOKB], out=tokb[:, :], in_=tokn[:, :])
        for grp in range(128 // GS):
            held = []
            for s in range(GS * grp, GS * grp + GS):
                u_, U_ = gather(j, s)
                held.append((s, u_, U_))
                if s % 3 == 0:
                    ji = jc[0] % 2
                    jc[0] += 1
                    vop("dve", "scalar_tensor_tensor", [U_, TOKN], [JUNKR[ji], DOTS[s]], out=junkr[ji][:, :], in0=u_[:, 0:2048], scalar=1.0, in1=tokn[:, :],
                        op0=ALU.mult, op1=ALU.mult, accum_out=dots[:, s:s + 1])
                else:
                    pi2 = pc[0] % 3
                    pc[0] += 1
                    vop("dve", "tensor_tensor", [U_, TOKB], [PROD[pi2]], out=prod[pi2][:, :], in0=u_[:, 0:2048], in1=tokb[:, :], op=ALU.mult)
                    ji2 = jc[1] % 3
                    jc[1] += 1
                    act(junk2r[ji2][:, :], prod[pi2][:, :], AF.Copy, [PROD[pi2]], [JUNK2R[ji2], DOTS[s]], accum_out=dots[:, s:s + 1])
            s0 = GS * grp
            act(coef[:, s0:s0 + GS], dots[:, s0:s0 + GS], AF.Gelu, [DOTS[s] for s in range(s0, s0 + GS)], [COEFG[grp]])
            vop("dve", "tensor_tensor", [COEFG[grp], GATB[j]], [COEFG[grp]], out=coef[:, s0:s0 + GS], in0=coef[:, s0:s0 + GS],
                in1=gates_all[:, j, s0:s0 + GS], op=ALU.mult)
            for (s, u_, U_) in held:
                di = s % NDG
                act(dg[di][:, :], ident, AF.Copy, [COEFG[grp], CONSTB], [DG[di]], scale=coef[:, s:s + 1])
                for n in range(4):
                    mm(pb[banks[n]][:, :], dg[di][:, :], u_[:, 2048 + 512 * n:2048 + 512 * (n + 1)], s == 0, s == 127, [DG[di], U_], [PB[banks[n]]])
        for n in range(4):
            vop("dve", "tensor_tensor", [H2T[k], PB[banks[n]]], [H2T[k]], out=h2t[k][:, 512 * n:512 * (n + 1)],
                in0=pb[banks[n]][:, :], in1=h2t[k][:, 512 * n:512 * (n + 1)], op=ALU.add)
        dma("sp", out_d[128 * j:128 * (j + 1), :], h2t[k][:, :], R=[H2T[k]], W=[OUTB[j]])
    return finish(nc, pg, dbg, {}, alloc, cur, cur[0], dma)


def finish(nc, pg, dbg, dumps, alloc, cur, scratch_at, dma):
    pg.barrier()
    for name, (t, dt, shape) in dumps.items():
        d = nc.dram_tensor("dbg_" + name, list(shape), dt, kind="ExternalOutput").ap()
        dma("sp", d, t[:], [], [])
        dbg[name] = True
    pg.barrier()
    pg.emit_all()
    return nc, dbg


def _consts():
    ident = np.eye(128, dtype=np.float32)
    j = np.arange(128)[:, None]
    s = np.arange(128)[None, :]
    negtri = np.where(j >= s, -1.0, 0.0).astype(np.float32)
    negones = -np.ones((128, 128), np.float32)
    q = np.arange(512)[None, None, :]
    jj = np.arange(4)[None, :, None]
    ss = np.arange(128)[:, None, None]
    masks = np.where(q > ss + 128 * jj, 0.0, NEG).astype(np.float32).reshape(128, 2048)
    cbf = np.concatenate([ident, negtri, negones, masks], axis=1).astype(ml_dtypes.bfloat16)
    cf32 = np.concatenate([np.ones((128, 128), np.float32), np.tile(np.arange(16, dtype=np.float32), (128, 1))], axis=1)
    return cbf, cf32


def _prep_shared(meta_tokens, norm_mix_g, w_in, conv_w, q_norm_g, k_norm_g, out_norm_g, w_out, norm_ffn_g, w_query, sub_keys):
    f = lambda a: np.ascontiguousarray(np.asarray(a, dtype=np.float32))
    cbf, cf32 = _consts()
    d = {
        "meta": f(meta_tokens),
        "gmix": f(norm_mix_g[0:1]),
        "gffn": f(norm_ffn_g[0:1]),
        "w_in_b": f(np.asarray(w_in[0]).reshape(16, 128, 48, 128).transpose(2, 1, 0, 3)),
        "cw": f(np.asarray(conv_w[0]).reshape(3, 8, 128).transpose(2, 1, 0).reshape(128, 24)),
        "gq": f(np.asarray(q_norm_g[0]).reshape(128, 1)),
        "gk": f(np.asarray(k_norm_g[0]).reshape(128, 1)),
        "ong": f(np.asarray(out_norm_g[0]).reshape(16, 128).T),
        "w_out_b": f(np.asarray(w_out[0]).reshape(16, 128, 2048).transpose(1, 0, 2)),
        "w_query_b": f(np.asarray(w_query[0]).reshape(16, 128, 2048).transpose(1, 0, 2)),
        "skT": f(np.asarray(sub_keys[0]).transpose(2, 0, 1).reshape(128, 256)),
        "cbf": cbf,
        "cf32": cf32,
    }
    return d


def kernel(x, meta_tokens, norm_mix_g, w_in, conv_w, q_norm_g, k_norm_g, out_norm_g, w_out,
           norm_ffn_g, w_query, sub_keys, expert_u, expert_v):
    nc, _ = build(99, True)
    shared = _prep_shared(meta_tokens, norm_mix_g, w_in, conv_w, q_norm_g, k_norm_g, out_norm_g, w_out, norm_ffn_g, w_query, sub_keys)
    shared["expert_u"] = np.ascontiguousarray(np.asarray(expert_u[0], dtype=np.float32))
    shared["expert_v"] = np.ascontiguousarray(np.asarray(expert_v[0], dtype=np.float32))
    x = np.asarray(x, dtype=np.float32)
    in_maps = []
    for b in range(8):
        m = dict(shared)
        m["x"] = np.ascontiguousarray(x[b])
        in_maps.append(m)
    res = run_bass_kernel_spmd(nc, in_maps, core_ids=list(range(8)))
    return np.stack([np.asarray(r["out"], dtype=np.float32) for r in res.results], axis=0)
```
